# Optimizing a Trainium2 kernel written in Bass

```python
import math
import jax, jax.numpy as jnp
from jax import lax
import numpy as np

D_MODEL = 1024
BATCH = 16
SEQ = 4096
DEPTH = 4

MIX_WIDTH = D_MODEL
DSA_HEADS = D_MODEL // 128
DSA_HEAD_DIM = 64
IDX_HEADS = 8
IDX_DIM = 64
TOPK_MAX = 256
DSA_Q_BLOCK = 64
DIFF_HEADS = D_MODEL // 256
DIFF_HEAD_DIM = 64
DIFF_V_DIM = 2 * DIFF_HEAD_DIM
DIFF_Q_BLOCK = 128
NUM_BUCKETS = 32
MAX_DISTANCE = 128
D_FF = ((8 * D_MODEL // 3 + 255) // 256) * 256
EPS = 1e-6

IN_WIDTHS = (
    DSA_HEADS * DSA_HEAD_DIM,
    DSA_HEADS * DSA_HEAD_DIM,
    DSA_HEADS * DSA_HEAD_DIM,
    IDX_HEADS * IDX_DIM,
    IDX_DIM,
    IDX_HEADS,
    DIFF_HEADS * 2 * DIFF_HEAD_DIM,
    DIFF_HEADS * 2 * DIFF_HEAD_DIM,
    DIFF_HEADS * DIFF_V_DIM,
)
IN_COLS = sum(IN_WIDTHS)
OUT_IN = DSA_HEADS * DSA_HEAD_DIM + DIFF_HEADS * DIFF_V_DIM

kernel_name = "hybrid_dsa_diffattn_parallel_heads"


def rms_norm(x, g):
    xf = x.astype(jnp.float32)
    y = xf * lax.rsqrt(jnp.mean(xf * xf, axis=-1, keepdims=True) + EPS)
    return (y * g.astype(jnp.float32)).astype(x.dtype)


def rel_bucket(dist):
    n = jnp.maximum(dist, 0)
    max_exact = NUM_BUCKETS // 2
    nf = jnp.maximum(n, 1).astype(jnp.float32)
    large = max_exact + (jnp.log(nf / max_exact) / math.log(MAX_DISTANCE / max_exact)
                         * (NUM_BUCKETS - max_exact)).astype(jnp.int32)
    large = jnp.minimum(large, NUM_BUCKETS - 1)
    return jnp.where(n < max_exact, n, large)


def split_cols(p, widths):
    outs, off = [], 0
    for w in widths:
        outs.append(p[..., off:off + w])
        off += w
    return outs


def dsa_mixer(q, k, v, q_idx, k_idx, w_idx, bias_tab):
    B, S, H, Dh = q.shape
    n_sel = min(TOPK_MAX, S // 4)
    n_blk = S // DSA_Q_BLOCK
    key_pos = jnp.arange(S, dtype=jnp.int32)

    def block(i):
        start = i * DSA_Q_BLOCK
        qb = lax.dynamic_slice_in_dim(q, start, DSA_Q_BLOCK, axis=1)
        qib = lax.dynamic_slice_in_dim(q_idx, start, DSA_Q_BLOCK, axis=1)
        wb = lax.dynamic_slice_in_dim(w_idx, start, DSA_Q_BLOCK, axis=1)
        q_pos = start + jnp.arange(DSA_Q_BLOCK, dtype=jnp.int32)
        dots = jnp.einsum('bqhd,bsd->bqhs', qib, k_idx) * (IDX_DIM ** -0.5)
        score = jnp.einsum('bqh,bqhs->bqs', wb * (IDX_HEADS ** -0.5), jax.nn.relu(dots))
        causal = key_pos[None, :] <= q_pos[:, None]
        score = jnp.where(causal[None], score.astype(jnp.float32), -jnp.inf)
        _, sel = lax.top_k(score, n_sel)
        valid = sel <= q_pos[None, :, None]
        flat = sel.reshape(B, -1)
        ks = jnp.take_along_axis(k, flat[:, :, None, None], axis=1).reshape(B, DSA_Q_BLOCK, n_sel, H, Dh)
        vs = jnp.take_along_axis(v, flat[:, :, None, None], axis=1).reshape(B, DSA_Q_BLOCK, n_sel, H, Dh)
        logits = jnp.einsum('bqhd,bqkhd->bqhk', qb, ks).astype(jnp.float32) * (Dh ** -0.5)
        bias = bias_tab[rel_bucket(q_pos[None, :, None] - sel)]
        logits = logits + jnp.transpose(bias, (0, 1, 3, 2)).astype(jnp.float32)
        logits = jnp.where(valid[:, :, None, :], logits, -jnp.inf)
        p = jax.nn.softmax(logits, axis=-1).astype(v.dtype)
        return jnp.einsum('bqhk,bqkhd->bqhd', p, vs)

    out = lax.map(block, jnp.arange(n_blk, dtype=jnp.int32))
    return jnp.transpose(out, (1, 0, 2, 3, 4)).reshape(B, S, H * Dh)


def diff_mixer(q, k, v, lam, lam_init, subln, bias_tab):
    B, S, Hd, _, Dd = q.shape
    n_blk = S // DIFF_Q_BLOCK
    key_pos = jnp.arange(S, dtype=jnp.int32)
    lamf = lam.astype(jnp.float32)
    lam_full = (jnp.exp(jnp.sum(lamf[0] * lamf[1])) - jnp.exp(jnp.sum(lamf[2] * lamf[3]))
                + lam_init)

    def block(i):
        start = i * DIFF_Q_BLOCK
        qb = lax.dynamic_slice_in_dim(q, start, DIFF_Q_BLOCK, axis=1)
        q_pos = start + jnp.arange(DIFF_Q_BLOCK, dtype=jnp.int32)
        logits = jnp.einsum('bqhcd,bshcd->bhcqs', qb, k).astype(jnp.float32) * (Dd ** -0.5)
        dist = q_pos[:, None] - key_pos[None, :]
        bias = jnp.transpose(bias_tab[rel_bucket(dist)], (2, 0, 1))
        logits = logits + bias[None, :, None].astype(jnp.float32)
        logits = jnp.where((dist >= 0)[None, None, None], logits, -jnp.inf)
        p = jax.nn.softmax(logits, axis=-1)
        attn = (p[:, :, 0] - lam_full * p[:, :, 1]).astype(v.dtype)
        return jnp.einsum('bhqs,bshe->bqhe', attn, v)

    out = lax.map(block, jnp.arange(n_blk, dtype=jnp.int32))
    out = jnp.transpose(out, (1, 0, 2, 3, 4)).reshape(B, S, Hd, 2 * Dd)
    out = rms_norm(out, subln) * (1.0 - lam_init)
    return out.reshape(B, S, Hd * 2 * Dd)


def setup_inputs(seed: int = 0) -> dict:
    key = jax.random.key(seed)
    ks = jax.random.split(key, 14)
    f32 = jnp.float32
    nrm = lambda k, shape, s: jax.random.normal(k, shape, f32) * s
    return {
        "x": nrm(ks[0], (BATCH, SEQ, D_MODEL), 1.0),
        "attn_norm": 1.0 + nrm(ks[1], (DEPTH, D_MODEL), 0.05),
        "w_in": nrm(ks[2], (DEPTH, D_MODEL, IN_COLS), D_MODEL ** -0.5),
        "diff_lambda": nrm(ks[3], (DEPTH, 4, DIFF_HEAD_DIM), 0.1),
        "diff_subln": 1.0 + nrm(ks[4], (DEPTH, DIFF_V_DIM), 0.05),
        "w_out": nrm(ks[5], (DEPTH, OUT_IN, D_MODEL), OUT_IN ** -0.5),
        "ffn_norm": 1.0 + nrm(ks[6], (DEPTH, D_MODEL), 0.05),
        "w_gate": nrm(ks[7], (DEPTH, D_MODEL, D_FF), D_MODEL ** -0.5),
        "w_up": nrm(ks[8], (DEPTH, D_MODEL, D_FF), D_MODEL ** -0.5),
        "w_down": nrm(ks[9], (DEPTH, D_FF, D_MODEL), D_FF ** -0.5),
        "rel_bias": nrm(ks[10], (NUM_BUCKETS, DSA_HEADS + DIFF_HEADS), 0.5),
        "final_norm": 1.0 + nrm(ks[11], (D_MODEL,), 0.05),
    }


def reference(x, attn_norm, w_in, diff_lambda, diff_subln, w_out, ffn_norm,
              w_gate, w_up, w_down, rel_bias, final_norm):
    B, S, _ = x.shape
    bias_dsa = rel_bias[:, :DSA_HEADS]
    bias_diff = rel_bias[:, DSA_HEADS:]
    for l in range(DEPTH):
        lam_init = 0.8 - 0.6 * math.exp(-0.3 * l)
        h = rms_norm(x, attn_norm[l])
        proj = jnp.einsum('bsd,dc->bsc', h, w_in[l])
        (dq, dk, dv, iq, ik, iw, fq, fk, fv) = split_cols(proj, IN_WIDTHS)
        dsa_out = dsa_mixer(
            dq.reshape(B, S, DSA_HEADS, DSA_HEAD_DIM),
            dk.reshape(B, S, DSA_HEADS, DSA_HEAD_DIM),
            dv.reshape(B, S, DSA_HEADS, DSA_HEAD_DIM),
            iq.reshape(B, S, IDX_HEADS, IDX_DIM), ik, iw, bias_dsa)
        diff_out = diff_mixer(
            fq.reshape(B, S, DIFF_HEADS, 2, DIFF_HEAD_DIM),
            fk.reshape(B, S, DIFF_HEADS, 2, DIFF_HEAD_DIM),
            fv.reshape(B, S, DIFF_HEADS, DIFF_V_DIM),
            diff_lambda[l], lam_init, diff_subln[l], bias_diff)
        mixed = jnp.concatenate([dsa_out, diff_out], axis=-1)
        x = x + jnp.einsum('bsc,cd->bsd', mixed, w_out[l])
        h = rms_norm(x, ffn_norm[l])
        g = jnp.einsum('bsd,df->bsf', h, w_gate[l])
        u = jnp.einsum('bsd,df->bsf', h, w_up[l])
        x = x + jnp.einsum('bsf,fd->bsd', jax.nn.silu(g) * u, w_down[l])
    return rms_norm(x, final_norm)
```

```python
import math
from contextlib import ExitStack
import numpy as np
import concourse.bass as bass
import concourse.mybir as mybir
from concourse.bass_utils import run_bass_kernel_spmd

F32 = mybir.dt.float32
BF16 = mybir.dt.bfloat16
AF = mybir.ActivationFunctionType
ALU = mybir.AluOpType
AX = mybir.AxisListType

D = 1024
DEPTH = 4
DFF = 2816
NFK = DFF // 128
INC = 3656
EPS = 1e-6
NEG = -30000.0
GW = 640
W0 = 1024.0
NB = 24
O_DQ, O_DK, O_DV, O_IQ, O_IK, O_IW, O_FQ, O_FK, O_FV = 0, 512, 1024, 1536, 2048, 2112, 2120, 2632, 3144


class Buf:
    __slots__ = ("name", "lw", "rd", "excl")

    def __init__(self, name, excl=False):
        self.name = name
        self.lw = None
        self.rd = {}
        self.excl = excl


class Res:
    def __init__(self, name, sem, step, handle=None):
        self.name, self.sem, self.step, self.h = name, sem, step, handle
        self.count = 0
        self.known = {}
        self.ops = []


class Builder:
    def __init__(self, S, layers, nseq, do_final, x_from_input=True):
        self.S, self.layers, self.nseq, self.do_final = S, layers, nseq, do_final
        self.NCH = S // 512
        self.NT = S // 128
        self.KSEL = min(256, S // 4)
        self.nc = bass.Bass("TRN2", target_bir_lowering=False)
        self.es = ExitStack()
        self.chans = {}
        self.nbuf = 0

    def sem(self, name):
        return self.es.enter_context(self.nc.semaphore(name))

    def chan(self, name):
        if name not in self.chans:
            self.chans[name] = Res(name, self.sem("c_" + name), 16)
        return self.chans[name]

    def buf(self, name="b"):
        self.nbuf += 1
        return Buf(f"{name}{self.nbuf}")

    def bufs(self, n, name="b"):
        return [self.buf(name) for _ in range(n)]

    def _wait(self, eng, deps):
        for (res, val) in deps:
            if eng.known.get(res, 0) >= val:
                continue
            eng.known[res] = val
            eng.ops.append(("w", res.sem, val * res.step))

    def _deps(self, eng, r, w, is_dma=False):
        deps = []
        for b in r:
            if b.lw is not None:
                deps.append(b.lw)
            if b.excl:
                for res, v in b.rd.items():
                    if res is not eng:
                        deps.append((res, v))
        for b in w:
            if b.lw is not None:
                deps.append(b.lw)
            for res, v in b.rd.items():
                deps.append((res, v))
        out = []
        for (res, v) in deps:
            if res is eng and (eng.name == "pe"):
                continue
            out.append((res, v))
        return out

    def E(self, eng, fn, r=(), w=()):
        deps = self._deps(eng, r, w)
        deps2 = []
        for (res, v) in deps:
            deps2.append((res, v))
        self._wait(eng, deps2)
        eng.count += 1
        eng.ops.append(("i", fn, eng.sem, 1))
        me = (eng, eng.count)
        for b in r:
            b.rd[eng] = eng.count
        for b in w:
            b.lw = me
            b.rd = {}

    def dma(self, out, in_, r, w, chan, q=None):
        ch = self.chan(chan)
        q = q or self.sp
        deps = self._deps(q, r, w)
        if ch.count > 0:
            deps.append((ch, ch.count))
        self._wait(q, deps)
        ch.count += 1
        q.ops.append(("i", (lambda h, o=out, i=in_: h.dma_start(out=o, in_=i)), ch.sem, 16))
        me = (ch, ch.count)
        for b in r:
            b.rd[ch] = ch.count
        for b in w:
            b.lw = me
            b.rd = {}

    def barrier(self):
        allres = self.engs + list(self.chans.values())
        for e in self.engs:
            self._wait(e, [(x, x.count) for x in allres if x is not e and x.count > 0])

    def sb(self, name, shape, dt, stack=None):
        self.nbuf += 1
        return (stack or self.es).enter_context(self.nc.sbuf_tensor(f"{name}_{self.nbuf}", shape, dt))

    def evac(self, idx, out, in_, r, w, scale=None):
        if idx % 2 == 0:
            self.E(self.act, lambda h: h.activation(out=out, in_=in_, func=AF.Copy), r=r, w=w)
        else:
            self.E(self.dve, lambda h: h.tensor_copy(out=out, in_=in_), r=r, w=w)

    def build(self):
        nc, S, NCH, NT = self.nc, self.S, self.NCH, self.NT
        L, NSEQ = len(self.layers), self.nseq
        es = self.es
        dt_in = lambda name, shape, dt=F32: nc.dram_tensor(name, shape, dt, kind="ExternalInput").ap()
        self.xT = dt_in("xT", [NSEQ, D, S])
        self.w_in = dt_in("w_in", [L, D, INC])
        self.w_out = dt_in("w_out", [L, D, D])
        self.w_gate = dt_in("w_gate", [L, D, DFF])
        self.w_up = dt_in("w_up", [L, D, DFF])
        self.w_down = dt_in("w_down", [L, DFF, D])
        self.gattn = dt_in("gattn", [L, 128, 8])
        self.gffn = dt_in("gffn", [L, 128, 8])
        self.gfin = dt_in("gfin", [128, 8])
        self.lamrep = dt_in("lamrep", [L, 128, 256])
        self.sublnrep = dt_in("sublnrep", [L, 128, 128])
        self.lconst = dt_in("lconst", [L, 128, 2])
        self.gtab = dt_in("gtab", [12, 128, GW])
        self.cb31 = dt_in("cb31", [128, 12])
        self.ident_in = dt_in("ident", [128, 128])
        self.causneg_in = dt_in("causneg", [128, 128])
        self.outT = nc.dram_tensor("outT", [NSEQ, D, S], F32, kind="ExternalOutput").ap()
        self.xs = nc.dram_tensor("xs", [NSEQ, D, S], F32).ap()
        self.win_fm = nc.dram_tensor("win_fm", [L, 128, 21, 8, 128], BF16).ap()
        self.win_v = nc.dram_tensor("win_v", [L, 128, 2, 8, 512], BF16).ap()
        self.win_iw = nc.dram_tensor("win_iw", [L, 128, 8, 8], BF16).ap()
        self.wo_s = nc.dram_tensor("wo_s", [L, 128, 8, 8, 128], BF16).ap()
        self.wg_s = nc.dram_tensor("wg_s", [L, 128, NFK, 8, 128], BF16).ap()
        self.wu_s = nc.dram_tensor("wu_s", [L, 128, NFK, 8, 128], BF16).ap()
        self.wd_s = nc.dram_tensor("wd_s", [L, 128, 8, NFK, 128], BF16).ap()
        self.dsaT_d = nc.dram_tensor("dsaT_d", [128, 4, S], BF16).ap()
        self.maskT_d = nc.dram_tensor("maskT_d", [NT, 128, 512], BF16).ap()

        mk = lambda name, h: Res(name, self.sem("s_" + name), 1, h)
        self.pe = mk("pe", nc.tensor)
        self.act = mk("act", nc.scalar)
        self.dve = mk("dve", nc.vector)
        self.pool = mk("pool", nc.gpsimd)
        self.sp = mk("sp", nc.sync)
        self.engs = [self.pe, self.act, self.dve, self.pool, self.sp]

        self.pb = [es.enter_context(nc.psum_tensor(f"pb{i}", [128, 512], F32)) for i in range(6)]
        self.pbB = [Buf(f"pb{i}", excl=True) for i in range(6)]
        self.ptp = [es.enter_context(nc.psum_tensor(f"ptp{i}", [128, 1024], BF16)) for i in range(2)]
        self.ptpB = [Buf(f"ptp{i}", excl=True) for i in range(2)]
        self.tpi = 0

        self.ident = self.sb("ident", [128, 128], BF16)
        self.ones = self.sb("ones", [128, 128], BF16)
        self.causneg = self.sb("causneg", [128, 128], F32)
        self.cb = self.sb("cb", [128, 12], F32)
        self.epsT = self.sb("epsT", [128, 1], F32)
        self.gA = self.sb("gA", [128, L, 8], F32)
        self.gF = self.sb("gF", [128, L, 8], F32)
        self.gFin = self.sb("gFin", [128, 8], F32)
        self.lam = self.sb("lam", [128, L, 256], F32)
        self.subln = self.sb("subln", [128, L, 128], F32)
        self.lc = self.sb("lc", [128, L, 2], F32)
        self.lamneg = self.sb("lamneg", [128, L], F32)
        self.CONST = self.buf("const")
        stage = self.sb("cstage", [128, 128], F32)
        C = self.CONST
        self.dma(stage[:], self.ident_in, [], [C], "c0")
        self.E(self.dve, lambda h: h.tensor_copy(out=self.ident[:], in_=stage[:]), r=[C], w=[C])
        self.E(self.dve, lambda h: h.memset(self.ones[:], 1.0), w=[C])
        self.E(self.dve, lambda h: h.memset(self.epsT[:], EPS), w=[C])
        self.dma(self.causneg[:], self.causneg_in, [], [C], "c0")
        self.dma(self.cb[:], self.cb31, [], [C], "c0")
        self.dma(self.gFin[:], self.gfin, [], [C], "c0")
        for l in range(L):
            self.dma(self.gA[:, l, :], self.gattn[l], [], [C], "c0")
            self.dma(self.gF[:, l, :], self.gffn[l], [], [C], "c0")
            self.dma(self.lam[:, l, :], self.lamrep[l], [], [C], "c0")
            self.dma(self.subln[:, l, :], self.sublnrep[l], [], [C], "c0")
            self.dma(self.lc[:, l, :], self.lconst[l], [], [C], "c0")
        lp = self.sb("lp", [128, 2, 64], F32)
        ls = self.sb("ls", [128, 2], F32)
        for l in range(L):
            lv = self.lam[:, l, :].rearrange("p (a b d) -> p a b d", a=2, b=2)
            self.E(self.dve, lambda h, lv=lv: h.tensor_tensor(out=lp[:], in0=lv[:, :, 0, :], in1=lv[:, :, 1, :], op=ALU.mult), r=[C], w=[C])
            self.E(self.dve, lambda h: h.tensor_reduce(out=ls[:], in_=lp[:], axis=AX.X, op=ALU.add), r=[C], w=[C])
            self.E(self.act, lambda h: h.activation(out=ls[:], in_=ls[:], func=AF.Exp), r=[C], w=[C])
            self.E(self.dve, lambda h, l=l: h.tensor_tensor(out=self.lamneg[:, l:l + 1], in0=ls[:, 1:2], in1=ls[:, 0:1], op=ALU.subtract), r=[C], w=[C])
            self.E(self.dve, lambda h, l=l: h.tensor_tensor(out=self.lamneg[:, l:l + 1], in0=self.lamneg[:, l:l + 1], in1=self.lc[:, l, 1:2], op=ALU.subtract), r=[C], w=[C])
        self.barrier()

        for li, l in enumerate(self.layers):
            self.phase0(li)
            for sq in range(NSEQ):
                src = self.xT if li == 0 else self.xs
                self.phase1(li, sq, src)
                self.phase2(li, sq, src, last=(li == L - 1))
        self.barrier()
        self.emit()
        return nc

    def phase0(self, li):
        nc = self.nc
        with ExitStack() as st:
            stg = [self.sb(f"p0s{i}", [128, INC], F32, st) for i in range(2)]
            cst = [self.sb(f"p0c{i}", [128, INC], BF16, st) for i in range(2)]
            sB, cB = self.bufs(2, "p0s"), self.bufs(2, "p0c")
            WS = self.buf("wscr")
            self.WS = WS
            k = [0]

            def one(src_rows, ncols, stores):
                i = k[0] % 2
                k[0] += 1
                self.dma(stg[i][:, 0:ncols], src_rows, [], [sB[i]], f"p0l{i}")
                if i == 0:
                    self.E(self.dve, lambda h: h.tensor_copy(out=cst[i][:, 0:ncols], in_=stg[i][:, 0:ncols]), r=[sB[i]], w=[cB[i]])
                else:
                    self.E(self.act, lambda h: h.activation(out=cst[i][:, 0:ncols], in_=stg[i][:, 0:ncols], func=AF.Copy), r=[sB[i]], w=[cB[i]])
                for si, (dst, c0, c1, wd) in enumerate(stores):
                    srcv = cst[i][:, c0:c1]
                    if wd is not None:
                        srcv = srcv.rearrange("p (n c) -> p n c", c=wd)
                    self.dma(dst, srcv, [cB[i]], [self.buf("wst")], f"p0st{i}_{si}")

            for rc in range(8):
                rows = slice(rc * 128, (rc + 1) * 128)
                fm = self.win_fm[li]
                one(self.w_in[li, rows, :], INC, [
                    (fm[:, 0:8, rc, :], O_DQ, O_DQ + 1024, 128),
                    (fm[:, 8:12, rc, :], O_IQ, O_IQ + 512, 128),
                    (fm[:, 12, rc, 0:64], O_IK, O_IK + 64, None),
                    (fm[:, 12, rc, 64:128], O_IK, O_IK + 64, None),
                    (fm[:, 13:21, rc, :], O_FQ, O_FQ + 1024, 128),
                    (self.win_v[li][:, 0, rc, :], O_DV, O_DV + 512, None),
                    (self.win_v[li][:, 1, rc, :], O_FV, O_FV + 512, None),
                    (self.win_iw[li][:, rc, :], O_IW, O_IW + 8, None),
                ])
                one(self.w_gate[li, rows, :], DFF, [(self.wg_s[li][:, :, rc, :], 0, DFF, 128)])
                one(self.w_up[li, rows, :], DFF, [(self.wu_s[li][:, :, rc, :], 0, DFF, 128)])
                one(self.w_out[li, rows, :], D, [(self.wo_s[li][:, :, rc, :], 0, D, 128)])
            for fk in range(NFK):
                rows = slice(fk * 128, (fk + 1) * 128)
                one(self.w_down[li, rows, :], D, [(self.wd_s[li][:, :, fk, :], 0, D, 128)])
            self.barrier()

    def norm(self, X, XB, g, H, HB, SQ, SQB, RS, RSB, out_f32=False):
        self.E(self.act, lambda h: h.activation(out=SQ, in_=X, func=AF.Square), r=[XB], w=[SQB])
        bi = self.nxt_bank()
        ps = self.pb[bi]
        for dk in range(8):
            self.E(self.pe, lambda h, dk=dk: h.matmul(ps[:], self.ones[:], SQ[:, dk, :], start=(dk == 0), stop=(dk == 7)),
                   r=[SQB, self.CONST], w=[self.pbB[bi]])
        self.E(self.act, lambda h: h.activation(out=RS, in_=ps[:], func=AF.Sqrt, bias=self.epsT[:], scale=1.0 / D),
               r=[self.pbB[bi], self.CONST], w=[RSB])
        self.E(self.dve, lambda h: h.reciprocal(out=RS, in_=RS), r=[RSB], w=[RSB])
        for dk in range(8):
            self.E(self.dve, lambda h, dk=dk: h.scalar_tensor_tensor(out=H[:, dk, :], in0=X[:, dk, :], scalar=g[:, dk:dk + 1], in1=RS,
                                                                      op0=ALU.mult, op1=ALU.mult),
                   r=[XB, RSB, self.CONST], w=[HB])

    def nxt_bank(self):
        self.bank_i = (getattr(self, "bank_i", -1) + 1) % len(self.mmbanks)
        return self.mmbanks[self.bank_i]

    def load_w(self, src, shape_key):
        slots = self.wslots[shape_key]
        i = slots["i"] % len(slots["t"])
        slots["i"] += 1
        t, b = slots["t"][i], slots["b"][i]
        self.dma(t[:], src, [self.WS], [b], f"w{shape_key}{i}")
        return t, b

    def proj_fm(self, li, tiles, H, HB, dests):
        for n, (ti, (dst, dB)) in enumerate(zip(tiles, dests)):
            wt, wb = self.load_w(self.win_fm[li][:, ti, :, :], "fm")
            bi = self.nxt_bank()
            ps = self.pb[bi]
            for dk in range(8):
                self.E(self.pe, lambda h, dk=dk, wt=wt, ps=ps: h.matmul(ps[:], wt[:, dk, :], H[:, dk, :], start=(dk == 0), stop=(dk == 7)),
                       r=[wb, HB], w=[self.pbB[bi]])
            self.evac(n, dst, ps[:], [self.pbB[bi]], [dB])

    def attention(self, j, groups, nhg, vw, use_mask, G, GB, li, P1):
        pass

    def phase1(self, li, sq, src):
        nc, S, NCH, NT = self.nc, self.S, self.NCH, self.NT
        with ExitStack() as st:
            kT = self.sb("kT", [128, 4, S], BF16, st)
            vA = self.sb("vA", [128, NT, 8, 65], BF16, st)
            kiT = self.sb("kiT", [128, S], BF16, st)
            XS = self.sb("XS", [128, 8 * 512], F32, st)
            SQM = self.sb("SQM", [128, 8 * 512], BF16, st)
            hT = self.sb("hT", [128, 8, 512], BF16, st)
            qT = self.sb("qT", [128, 4, 512], BF16, st)
            iqT = self.sb("iqT", [128, 4, 512], BF16, st)
            Gt = self.sb("Gt", [128, 4, GW], F32, st)
            RS = self.sb("RS", [128, 512], F32, st)
            wsb = self.sb("wsb", [128, 4, 8], F32, st)
            Rt = [self.sb(f"Rt{i}", [128, 512], BF16, st) for i in range(4)]
            Pt = [self.sb(f"Pt{i}", [128, 512], BF16, st) for i in range(4)]
            Pm = [self.sb(f"Pm{i}", [128, 512], BF16, st) for i in range(4)]
            Tm = [self.sb(f"Tm{i}", [128, 512], F32, st) for i in range(2)]
            mst = [self.sb(f"mst{i}", [128, 4, 128], BF16, st) for i in range(2)]
            mT = [self.sb(f"mT{i}", [128, 512], BF16, st) for i in range(2)]
            osb = self.sb("osb", [128, 4, 512], BF16, st)
            dsT = self.sb("dsT", [128, 4, 512], BF16, st)
            rc = self.sb("rc", [128, 4], F32, st)
            bis = self.sb("bis", [128, 8], F32, st)
            XS_b = self.sb("XSb", [128, 8 * 512], F32, st)
            SQM_b = self.sb("SQMb", [128, 8 * 512], BF16, st)
            bis_b = self.sb("bisb", [128, 8], F32, st)
            dgs = [[self.sb(f"dg{q}{i}", [128, 128], BF16, st) for i in range(4)] for q in range(2)]
            wfm = [self.sb(f"wfm{i}", [128, 8, 128], BF16, st) for i in range(3)]
            wv = [self.sb(f"wv{i}", [128, 8, 512], BF16, st) for i in range(1)]
            wiw = [self.sb(f"wiw{i}", [128, 8, 8], BF16, st) for i in range(1)]
            self.wslots = {"fm": {"t": wfm, "b": self.bufs(3), "i": 0}, "v": {"t": wv, "b": self.bufs(1), "i": 0},
                           "iw": {"t": wiw, "b": self.bufs(1), "i": 0}}
            B = lambda n="p1": self.buf(n)
            kTB, vAB, kiTB = self.bufs(NCH), self.bufs(NCH), self.bufs(NCH)
            XSB, SQMB, hTB, qTB, iqTB, GB, RSB, wsbB = B(), B(), B(), B(), B(), B(), B(), B()
            RtB, PtB, PmB, TmB, mstB, mTB = self.bufs(4), self.bufs(4), self.bufs(4), self.bufs(2), self.bufs(2), self.bufs(2)
            osbB, dsTB, rcB, bisB = B(), B(), B(), B()
            XSs, SQMs, biss = [XS, XS_b], [SQM, SQM_b], [bis, bis_b]
            XSBs, SQMBs, SQABs = [XSB, B()], [SQMB, B()], [B(), B()]
            midBs, cntBs, sgBs, tmpBs, dgBs = self.bufs(2), self.bufs(2), self.bufs(2), self.bufs(2), self.bufs(2)
            mdB = self.buf("maskT_d")
            dsdB = self.dsdB = self.bufs(NCH, "dsaTd")
            Xv = XS[:].rearrange("p (k t) -> p k t", k=8)
            SQv = SQM[:].rearrange("p (k t) -> p k t", k=8)
            self.E(self.pool, lambda h: h.memset(vA[:, :, :, 64:65], 1.0), w=vAB)
            cnt, tmp, mid, theta, sg = bis[:, 0:1], bis[:, 1:2], bis[:, 2:3], bis[:, 3:4], bis[:, 4:5]
            xsrc = src[sq].rearrange("(k p) t -> p k t", p=128)
            for j in range(NCH):
                t0 = 512 * j
                self.mmbanks = [0, 1, 2, 3, 4, 5]
                self.dma(Xv, xsrc[:, :, t0:t0 + 512], [self.xB[sq][j]], [XSB], "x")
                self.norm(Xv, XSB, self.gA[:, li, :], hT[:], hTB, SQv, SQMB, RS[:], RSB)
                dests = [(qT[:, c, :], qTB) for c in range(4)] + [(kT[:, c, t0:t0 + 512], kTB[j]) for c in range(4)] + \
                        [(iqT[:, c, :], iqTB) for c in range(4)] + [(kiT[:, t0:t0 + 512], kiTB[j])]
                self.proj_fm(li, list(range(13)), hT, hTB, dests)
                wt, wb = self.load_w(self.win_v[li][:, 0, :, :], "v")
                wi, wib = self.load_w(self.win_iw[li], "iw")
                for tt in range(4):
                    bi = self.nxt_bank()
                    ps = self.pb[bi]
                    for dk in range(8):
                        self.E(self.pe, lambda h, dk=dk, tt=tt, ps=ps, wt=wt: h.matmul(ps[:], hT[:, dk, tt * 128:(tt + 1) * 128], wt[:, dk, :],
                                                                              start=(dk == 0), stop=(dk == 7)), r=[wb, hTB], w=[self.pbB[bi]])
                    self.evac(tt, vA[:, 4 * j + tt, :, 0:64], ps[:].rearrange("p (h e) -> p h e", h=8), [self.pbB[bi]], [vAB[j]])
                    bi = self.nxt_bank()
                    ps2 = self.pb[bi]
                    for dk in range(8):
                        self.E(self.pe, lambda h, dk=dk, tt=tt, ps2=ps2, wi=wi: h.matmul(ps2[:, 0:8], hT[:, dk, tt * 128:(tt + 1) * 128], wi[:, dk, :],
                                                                                start=(dk == 0), stop=(dk == 7)), r=[wib, hTB], w=[self.pbB[bi]])
                    self.E(self.dve, lambda h, tt=tt, ps2=ps2: h.tensor_copy(out=wsb[:, tt, :], in_=ps2[:, 0:8]), r=[self.pbB[bi]], w=[wsbB])
                for pair in ((0, 1), (2, 3)):
                    for sl, qi in enumerate(pair):
                        i = 4 * j + qi
                        XSc, XSBc = XSs[sl], XSBs[sl]
                        dgc, dgBc = dgs[sl], dgBs[sl]
                        for a in range(4):
                            self.E(self.dve, lambda h, a=a, qi=qi, dgc=dgc: h.tensor_scalar(out=dgc[a][:], in0=self.ident[:], scalar1=wsb[:, qi, 2 * a + 1:2 * a + 2], scalar2=None, op0=ALU.mult),
                                   r=[self.CONST, wsbB], w=[dgBc])
                        ib = [(kb, hd) for kb in range(j + 1) for hd in range(8)]
                        ILAG = 2

                        def ifront(n, qi=qi, j=j, ib=ib):
                            kb, hd = ib[n]
                            wdt = 512 if kb < j else 128 * (qi + 1)
                            pr = slice((hd % 2) * 64, (hd % 2) * 64 + 64)
                            db = (0, 1, 2, 5)[n % 4]
                            dps = self.pb[db]
                            self.E(self.pe, lambda h, pr=pr, hd=hd, dps=dps, kb=kb, wdt=wdt, qi=qi: h.matmul(
                                dps[:, 0:wdt], iqT[pr, hd // 2, qi * 128:(qi + 1) * 128], kiT[pr, kb * 512:kb * 512 + wdt], start=True, stop=True),
                                r=[iqTB] + kiTB[:j + 1], w=[self.pbB[db]])
                            ri = n % 4
                            if hd % 2 == 0:
                                self.E(self.dve, lambda h, dps=dps, ri=ri, wdt=wdt, qi=qi, hd=hd: h.tensor_scalar(
                                    out=Rt[ri][:, 0:wdt], in0=dps[:, 0:wdt], scalar1=0.0, scalar2=wsb[:, qi, hd:hd + 1], op0=ALU.max, op1=ALU.mult),
                                    r=[self.pbB[db], wsbB], w=[RtB[ri]])
                            else:
                                self.E(self.act, lambda h, dps=dps, ri=ri, wdt=wdt: h.activation(out=Rt[ri][:, 0:wdt], in_=dps[:, 0:wdt], func=AF.Relu),
                                       r=[self.pbB[db]], w=[RtB[ri]])

                        def iback(n, qi=qi, j=j, ib=ib, XSc=XSc, XSBc=XSBc, dgc=dgc, dgBc=dgBc):
                            kb, hd = ib[n]
                            wdt = 512 if kb < j else 128 * (qi + 1)
                            sbank = 3 + (kb % 2)
                            sps = self.pb[sbank]
                            ri = n % 4
                            if hd % 2 == 0:
                                self.E(self.pe, lambda h, sps=sps, ri=ri, wdt=wdt, hd=hd: h.matmul(
                                    sps[:, 0:wdt], self.ident[:], Rt[ri][:, 0:wdt], start=(hd == 0), stop=(hd == 7)),
                                    r=[RtB[ri], self.CONST], w=[self.pbB[sbank]])
                            else:
                                self.E(self.pe, lambda h, sps=sps, ri=ri, wdt=wdt, hd=hd: h.matmul(
                                    sps[:, 0:wdt], dgc[hd // 2][:], Rt[ri][:, 0:wdt], start=(hd == 0), stop=(hd == 7)),
                                    r=[RtB[ri], dgBc], w=[self.pbB[sbank]])
                            if hd == 7:
                                self.E(self.act, lambda h, sps=sps, kb=kb, wdt=wdt: h.activation(out=XSc[:, kb * 512:kb * 512 + wdt], in_=sps[:, 0:wdt], func=AF.Copy),
                                       r=[self.pbB[sbank]], w=[XSBc])

                        for n in range(0, len(ib) + ILAG, 2):
                            for m in (n, n + 1):
                                if m < len(ib):
                                    ifront(m)
                            for m in (n, n + 1):
                                if ILAG <= m < len(ib) + ILAG:
                                    iback(m - ILAG)
                        self.E(self.dve, lambda h, i=i, XSc=XSc: h.tensor_tensor(out=XSc[:, i * 128:(i + 1) * 128], in0=XSc[:, i * 128:(i + 1) * 128], in1=self.causneg[:], op=ALU.add),
                               r=[XSBc, self.CONST], w=[XSBc])
                    tiles = []
                    for sl, qi in enumerate(pair):
                        i = 4 * j + qi
                        nkeys = 128 * (i + 1)
                        bb = biss[sl]
                        tiles.append(dict(sl=sl, qi=qi, i=i, nkeys=nkeys, XS=XSs[sl], XSB=XSBs[sl], SQM=SQMs[sl], SQMB=SQMBs[sl], SQAB=SQABs[sl],
                                          cnt=bb[:, 0:1], tmp=bb[:, 1:2], mid=bb[:, 2:3], theta=bb[:, 3:4], sg=bb[:, 4:5],
                                          midB=midBs[sl], cntB=cntBs[sl], sgB=sgBs[sl], tmpB=tmpBs[sl],
                                          nD=max(128, int(round(0.44 * nkeys / 128)) * 128)))
                    for t in tiles:
                        if t["nkeys"] > self.KSEL:
                            self.E(self.dve, lambda h, t=t: h.memset(t["mid"], 0.0), w=[t["midB"]])
                        else:
                            self.E(self.dve, lambda h, t=t: h.memset(t["theta"], -W0), w=[t["midB"]])
                    for k in range(1, NB + 1):
                        c = W0 / 2 ** (k + 1) if k < NB else W0 / 2 ** NB
                        m2 = 2 * c if k < NB else c
                        for t in tiles:
                            if t["nkeys"] <= self.KSEL:
                                continue
                            nD, nkeys = t["nD"], t["nkeys"]
                            nA = nkeys - nD
                            dst = t["mid"] if k < NB else t["theta"]
                            self.E(self.dve, lambda h, t=t, nD=nD: h.tensor_scalar(out=t["SQM"][:, 0:nD], in0=t["XS"][:, 0:nD], scalar1=t["mid"], scalar2=None,
                                                                                   op0=ALU.is_ge, op1=ALU.add, accum_out=t["cnt"]), r=[t["XSB"], t["midB"]], w=[t["SQMB"], t["cntB"]])
                            self.E(self.act, lambda h, t=t, nD=nD, nkeys=nkeys: h.activation(out=t["SQM"][:, nD:nkeys], in_=t["XS"][:, nD:nkeys], func=AF.Sign, bias=t["mid"], scale=-1.0,
                                                                                             accum_out=t["sg"]), r=[t["XSB"], t["midB"]], w=[t["SQAB"], t["sgB"]])
                            self.E(self.dve, lambda h, t=t: h.scalar_tensor_tensor(out=t["tmp"], in0=t["sg"], scalar=-0.5, in1=t["cnt"], op0=ALU.mult, op1=ALU.add),
                                   r=[t["sgB"], t["cntB"]], w=[t["tmpB"]])
                            self.E(self.dve, lambda h, t=t, m2=m2, nA=nA: h.tensor_scalar(out=t["tmp"], in0=t["tmp"], scalar1=self.KSEL - 0.5 - nA / 2.0, scalar2=m2, op0=ALU.is_ge, op1=ALU.mult),
                                   r=[t["tmpB"]], w=[t["tmpB"]])
                            self.E(self.dve, lambda h, t=t, c=c, dst=dst: h.scalar_tensor_tensor(out=dst, in0=t["tmp"], scalar=-c, in1=t["mid"], op0=ALU.add, op1=ALU.add),
                                   r=[t["tmpB"], t["midB"]], w=[t["midB"]])
                    for t in tiles:
                        nkeys, qi, i = t["nkeys"], t["qi"], t["i"]
                        SQc, SQBc = t["SQM"], t["SQMB"]
                        self.E(self.dve, lambda h, t=t, nkeys=nkeys: h.tensor_scalar(out=t["SQM"][:, 0:nkeys], in0=t["XS"][:, 0:nkeys], scalar1=t["theta"], scalar2=None, op0=ALU.is_ge),
                               r=[t["XSB"], t["midB"]], w=[t["SQMB"], t["SQAB"]])
                        for g0 in range(0, i + 1, 4):
                            n = min(4, i + 1 - g0)
                            tp = self.tpi % 2
                            self.tpi += 1
                            for a in range(n):
                                stt = g0 + a
                                self.E(self.pe, lambda h, tp=tp, a=a, stt=stt, SQc=SQc: h.transpose(self.ptp[tp][:, a * 128:(a + 1) * 128], SQc[:, stt * 128:(stt + 1) * 128], self.ident[:]),
                                       r=[SQBc, self.CONST], w=[self.ptpB[tp]])
                            self.E(self.act, lambda h, tp=tp, n=n: h.activation(out=mst[tp][:, 0:n, :], in_=self.ptp[tp][:, 0:n * 128].rearrange("p (n c) -> p n c", c=128), func=AF.Copy),
                                   r=[self.ptpB[tp]], w=[mstB[tp]])
                            self.dma(self.maskT_d[g0:g0 + n, :, qi * 128:(qi + 1) * 128].rearrange("n s t -> s n t"), mst[tp][:, 0:n, :], [mstB[tp]], [mdB], f"mst{tp}")
                for hg in range(2):
                    self.dma(Gt[:], self.gtab[4 * hg:4 * hg + 4].rearrange("g p c -> p g c"), [], [GB], "G")
                    heads = []
                    for hh in range(4):
                        hd = 4 * hg + hh
                        pr = slice((hd % 2) * 64, (hd % 2) * 64 + 64)
                        heads.append(dict(
                            k=lambda s0, pr=pr, hd=hd: kT[pr, hd // 2, s0:s0 + 128],
                            q=lambda c0, pr=pr, hd=hd: qT[pr, hd // 2, c0:512],
                            gi=hh, cb=self.cb[:, hd:hd + 1],
                            v=lambda stt, hd=hd: vA[:, stt, hd, :], os=hh))
                    def fin(qi, hg=hg):
                        ops = self.pb[qi]
                        ov = ops[:, 0:260].rearrange("p (h e) -> p h e", e=65)
                        self.E(self.dve, lambda h: h.reciprocal(out=rc[:], in_=ov[:, :, 64]), r=[self.pbB[qi]], w=[rcB])
                        for hh in range(4):
                            hd = 4 * hg + hh
                            self.E(self.dve, lambda h, hh=hh, hd=hd: h.tensor_scalar(out=osb[:, qi, hd * 64:(hd + 1) * 64], in0=ov[:, hh, 0:64], scalar1=rc[:, hh:hh + 1],
                                                                                     scalar2=None, op0=ALU.mult), r=[self.pbB[qi], rcB], w=[osbB])
                        for ft in (2 * hg, 2 * hg + 1):
                            tp = self.tpi % 2
                            self.tpi += 1
                            self.E(self.pe, lambda h, tp=tp, ft=ft: h.transpose(self.ptp[tp][:, 0:128], osb[:, qi, ft * 128:(ft + 1) * 128], self.ident[:]),
                                   r=[osbB, self.CONST], w=[self.ptpB[tp]])
                            self.E(self.act, lambda h, tp=tp, ft=ft: h.activation(out=dsT[:, ft, qi * 128:(qi + 1) * 128], in_=self.ptp[tp][:, 0:128], func=AF.Copy),
                                   r=[self.ptpB[tp]], w=[dsTB])
                    self.attn_group(j, heads, 65, (mT, mTB, mdB), Gt, GB, Pt, PtB, Pm, PmB, Tm, TmB, kTB, vAB, qTB, fin)
                self.dma(self.dsaT_d[:, :, t0:t0 + 512], dsT[:], [dsTB], [dsdB[j]], "dsst")
            self.barrier()

    def attn_group(self, j, heads, vw1, mask, Gt, GB, Pt, PtB, Pm, PmB, Tm, TmB, kB, vB, qB, fin):
        nst = 4 * j + 4
        base = getattr(self, "_ac", 0)
        blocks = [(stt, hd) for stt in range(nst) for hd in heads]
        LAG = 2
        st_ = {}

        def front(n):
            stt, hd = blocks[n]
            g = base + n
            s0 = 128 * stt
            r = 4 * j - stt
            col0 = 0 if r >= 0 else -128 * r
            N = 512 - col0
            near = r <= 1
            mi = stt % 2
            if mask is not None and hd is heads[0]:
                mT, mTB, mdB = mask
                self.dma(mT[mi][:, col0:512], self.maskT_d[stt][:, col0:512], [mdB], [mTB[mi]], f"mT{mi}")
            sb = 4 + g % 2
            pi = g % 4
            sps = self.pb[sb]
            self.E(self.pe, lambda h, hd=hd, sps=sps, s0=s0, col0=col0, N=N: h.matmul(sps[:, 0:N], hd["k"](s0), hd["q"](col0), start=True, stop=True),
                   r=[qB] + kB[:j + 1], w=[self.pbB[sb]])
            if near:
                ti = g % 2
                g0 = 128 * r + col0
                self.E(self.dve, lambda h, hd=hd, sps=sps, ti=ti, N=N, g0=g0: h.scalar_tensor_tensor(
                    out=Tm[ti][:, 0:N], in0=sps[:, 0:N], scalar=0.125, in1=Gt[:, hd["gi"], g0:g0 + N], op0=ALU.mult, op1=ALU.add),
                    r=[self.pbB[sb], GB], w=[TmB[ti]])
                self.E(self.act, lambda h, ti=ti, pi=pi, N=N: h.activation(out=Pt[pi][:, 0:N], in_=Tm[ti][:, 0:N], func=AF.Exp),
                       r=[TmB[ti]], w=[PtB[pi]])
            else:
                self.E(self.act, lambda h, hd=hd, sps=sps, pi=pi, N=N: h.activation(out=Pt[pi][:, 0:N], in_=sps[:, 0:N], func=AF.Exp, bias=hd["cb"], scale=0.125),
                       r=[self.pbB[sb], self.CONST], w=[PtB[pi]])
            if mask is not None:
                mT, mTB, mdB = mask
                eng = self.pool if g % 3 == 0 else self.dve
                self.E(eng, lambda h, pi=pi, mi=mi, N=N, col0=col0, mT=mT: h.tensor_tensor(out=Pm[pi][:, 0:N], in0=Pt[pi][:, 0:N], in1=mT[mi][:, col0:512], op=ALU.mult),
                       r=[PtB[pi], mTB[mi]], w=[PmB[pi]])
                st_[n] = (Pm[pi], PmB[pi], col0)
            else:
                st_[n] = (Pt[pi], PtB[pi], col0)

        def back(n):
            stt, hd = blocks[n]
            PP, PPB, col0 = st_.pop(n)
            for qi in range(col0 // 128, 4):
                last = 4 * j + qi
                o = self.pb[qi][:, hd["os"] * vw1:(hd["os"] + 1) * vw1]
                self.E(self.pe, lambda h, o=o, PP=PP, qi=qi, col0=col0, hd=hd, stt=stt, last=last: h.matmul(
                    o, PP[:, qi * 128 - col0:qi * 128 - col0 + 128], hd["v"](stt), start=(stt == 0 and hd is heads[0]), stop=(stt == last and hd is heads[-1])),
                    r=[PPB] + vB[:j + 1], w=[self.pbB[qi]])

        for n in range(0, len(blocks) + LAG, 2):
            for m in (n, n + 1):
                if m < len(blocks):
                    front(m)
            for m in (n, n + 1):
                if LAG <= m < len(blocks) + LAG:
                    back(m - LAG)
        cnt = base + len(blocks)
        self._ac = cnt
        for qi in range(4):
            fin(qi)

    def phase2(self, li, sq, src, last):
        nc, S, NCH, NT = self.nc, self.S, self.NCH, self.NT
        with ExitStack() as st:
            kT = self.sb("fkT", [128, 4, S], BF16, st)
            vA = self.sb("fvA", [128, NT, 4, 129], BF16, st)
            XS = self.sb("XS2", [128, 8, 512], F32, st)
            ACT_T = self.sb("ACTT", [128, NFK, 512], BF16, st)
            hT = self.sb("hT2", [128, 8, 512], BF16, st)
            qT = self.sb("fqT", [128, 4, 512], BF16, st)
            Gt = self.sb("Gt2", [128, 4, GW], F32, st)
            RS = self.sb("RS2", [128, 512], F32, st)
            mix = self.sb("mix", [128, 8, 512], BF16, st)
            Pt = [self.sb(f"Pu{i}", [128, 512], BF16, st) for i in range(4)]
            Tm = [self.sb(f"Tn{i}", [128, 512], F32, st) for i in range(2)]
            osb = self.sb("osb2", [128, 4, 512], BF16, st)
            o1 = self.sb("o1", [128, 128], F32, st)
            dd = self.sb("dd", [128, 128], F32, st)
            jk = self.sb("jk", [128, 128], F32, st)
            sm = self.sb("sm", [128, 8], F32, st)
            sil = [self.sb(f"sil{i}", [128, 512], F32, st) for i in range(2)]
            OF = self.sb("OF", [128, 8, 512], F32, st) if (last and self.do_final) else None
            wfm = [self.sb(f"xfm{i}", [128, 8, 128], BF16, st) for i in range(4)]
            wv = [self.sb(f"xv{i}", [128, 8, 512], BF16, st) for i in range(1)]
            wd = [self.sb(f"xd{i}", [128, NFK, 128], BF16, st) for i in range(2)]
            self.wslots = {"fm": {"t": wfm, "b": self.bufs(4), "i": 0}, "v": {"t": wv, "b": self.bufs(1), "i": 0},
                           "d": {"t": wd, "b": self.bufs(2), "i": 0}}
            B = lambda n="p2": self.buf(n)
            kTB, vAB = self.bufs(NCH), self.bufs(NCH)
            XSB, ACTB, hTB, qTB, GB, RSB, mixB, osbB, smB, OFB = B(), B(), B(), B(), B(), B(), B(), B(), B(), B()
            PtB, TmB, silB = self.bufs(4), self.bufs(2), self.bufs(2)
            SQv = ACT_T[:, 0:8, :]
            self.E(self.pool, lambda h: h.memset(vA[:, :, :, 128:129], 1.0), w=vAB)
            self.dma(Gt[:], self.gtab[8:12].rearrange("g p c -> p g c"), [], [GB], "G")
            xsrc = src[sq].rearrange("(k p) t -> p k t", p=128)
            xdst = self.xs[sq].rearrange("(k p) t -> p k t", p=128)
            odst = self.outT[sq].rearrange("(k p) t -> p k t", p=128)
            r1, r2, ssq, rstd = sm[:, 0:1], sm[:, 1:2], sm[:, 2:3], sm[:, 3:4]
            for j in range(NCH):
                t0 = 512 * j
                self.mmbanks = [0, 1, 2, 3, 4, 5]
                self.dma(XS[:], xsrc[:, :, t0:t0 + 512], [self.xB[sq][j]], [XSB], "x")
                self.norm(XS[:], XSB, self.gA[:, li, :], hT[:], hTB, SQv, ACTB, RS[:], RSB)
                dests = [(qT[:, c, :], qTB) for c in range(4)] + [(kT[:, c, t0:t0 + 512], kTB[j]) for c in range(4)]
                self.proj_fm(li, list(range(13, 21)), hT, hTB, dests)
                wt, wb = self.load_w(self.win_v[li][:, 1, :, :], "v")
                for tt in range(4):
                    bi = self.nxt_bank()
                    ps = self.pb[bi]
                    for dk in range(8):
                        self.E(self.pe, lambda h, dk=dk, tt=tt, ps=ps, wt=wt: h.matmul(ps[:], hT[:, dk, tt * 128:(tt + 1) * 128], wt[:, dk, :],
                                                                              start=(dk == 0), stop=(dk == 7)), r=[wb, hTB], w=[self.pbB[bi]])
                    self.evac(tt, vA[:, 4 * j + tt, :, 0:128], ps[:].rearrange("p (h e) -> p h e", h=4), [self.pbB[bi]], [vAB[j]])
                self.dma(mix[:, 0:4, :], self.dsaT_d[:, :, t0:t0 + 512], [self.dsdB[j]], [mixB], "mixl")
                for hh in range(4):
                    heads = []
                    for c in range(2):
                        pr = slice(c * 64, c * 64 + 64)
                        heads.append(dict(
                            k=lambda s0, pr=pr, hh=hh: kT[pr, hh, s0:s0 + 128],
                            q=lambda c0, pr=pr, hh=hh: qT[pr, hh, c0:512],
                            gi=hh, cb=self.cb[:, 8 + hh:9 + hh],
                            v=lambda stt, hh=hh: vA[:, stt, hh, :], os=c))
                    def fin(qi, hh=hh):
                        ops = self.pb[qi]
                        ov = ops[:, 0:258].rearrange("p (c e) -> p c e", e=129)
                        self.E(self.dve, lambda h: h.reciprocal(out=sm[:, 0:2], in_=ov[:, :, 128]), r=[self.pbB[qi]], w=[smB])
                        self.E(self.dve, lambda h: h.tensor_tensor(out=r2, in0=r2, in1=self.lamneg[:, li:li + 1], op=ALU.mult), r=[smB, self.CONST], w=[smB])
                        self.E(self.dve, lambda h: h.tensor_scalar(out=o1[:], in0=ov[:, 0, 0:128], scalar1=r1, scalar2=None, op0=ALU.mult), r=[self.pbB[qi], smB], w=[smB])
                        self.E(self.dve, lambda h: h.scalar_tensor_tensor(out=dd[:], in0=ov[:, 1, 0:128], scalar=r2, in1=o1[:], op0=ALU.mult, op1=ALU.add),
                               r=[self.pbB[qi], smB], w=[smB])
                        self.E(self.act, lambda h: h.activation(out=jk[:], in_=dd[:], func=AF.Square, accum_out=ssq), r=[smB], w=[smB])
                        self.E(self.act, lambda h: h.activation(out=rstd, in_=ssq, func=AF.Sqrt, bias=self.epsT[:], scale=1.0 / 128), r=[smB, self.CONST], w=[smB])
                        self.E(self.dve, lambda h: h.reciprocal(out=rstd, in_=rstd), r=[smB], w=[smB])
                        self.E(self.dve, lambda h: h.tensor_tensor(out=rstd, in0=rstd, in1=self.lc[:, li, 0:1], op=ALU.mult), r=[smB, self.CONST], w=[smB])
                        self.E(self.dve, lambda h: h.scalar_tensor_tensor(out=osb[:, qi, hh * 128:(hh + 1) * 128], in0=dd[:], scalar=rstd, in1=self.subln[:, li, :],
                                                                          op0=ALU.mult, op1=ALU.mult), r=[smB, self.CONST], w=[osbB])
                        tp = self.tpi % 2
                        self.tpi += 1
                        self.E(self.pe, lambda h, tp=tp: h.transpose(self.ptp[tp][:, 0:128], osb[:, qi, hh * 128:(hh + 1) * 128], self.ident[:]),
                               r=[osbB, self.CONST], w=[self.ptpB[tp]])
                        self.E(self.act, lambda h, tp=tp: h.activation(out=mix[:, 4 + hh, qi * 128:(qi + 1) * 128], in_=self.ptp[tp][:, 0:128], func=AF.Copy),
                               r=[self.ptpB[tp]], w=[mixB])
                    self.attn_group(j, heads, 129, None, Gt, GB, Pt, PtB, None, None, Tm, TmB, kTB, vAB, qTB, fin)
                for dt_ in range(8):
                    wt, wb = self.load_w(self.wo_s[li][:, dt_, :, :], "fm")
                    bi = self.nxt_bank()
                    ps = self.pb[bi]
                    for ck in range(8):
                        self.E(self.pe, lambda h, ck=ck, wt=wt, ps=ps: h.matmul(ps[:], wt[:, ck, :], mix[:, ck, :], start=(ck == 0), stop=(ck == 7)),
                               r=[wb, mixB], w=[self.pbB[bi]])
                    self.E(self.dve, lambda h, dt_=dt_, ps=ps: h.tensor_tensor(out=XS[:, dt_, :], in0=XS[:, dt_, :], in1=ps[:], op=ALU.add),
                           r=[self.pbB[bi], XSB], w=[XSB])
                self.norm(XS[:], XSB, self.gF[:, li, :], hT[:], hTB, SQv, ACTB, RS[:], RSB)
                for ft in range(NFK):
                    wg, wgb = self.load_w(self.wg_s[li][:, ft, :, :], "fm")
                    wu, wub = self.load_w(self.wu_s[li][:, ft, :, :], "fm")
                    bg = self.nxt_bank()
                    bu = self.nxt_bank()
                    pg, pu = self.pb[bg], self.pb[bu]
                    for dk in range(8):
                        self.E(self.pe, lambda h, dk=dk, wg=wg, pg=pg: h.matmul(pg[:], wg[:, dk, :], hT[:, dk, :], start=(dk == 0), stop=(dk == 7)),
                               r=[wgb, hTB], w=[self.pbB[bg]])
                    for dk in range(8):
                        self.E(self.pe, lambda h, dk=dk, wu=wu, pu=pu: h.matmul(pu[:], wu[:, dk, :], hT[:, dk, :], start=(dk == 0), stop=(dk == 7)),
                               r=[wub, hTB], w=[self.pbB[bu]])
                    si = ft % 2
                    self.E(self.act, lambda h, si=si, pg=pg: h.activation(out=sil[si][:], in_=pg[:], func=AF.Silu), r=[self.pbB[bg]], w=[silB[si]])
                    self.E(self.dve, lambda h, si=si, pu=pu, ft=ft: h.tensor_tensor(out=ACT_T[:, ft, :], in0=sil[si][:], in1=pu[:], op=ALU.mult),
                           r=[silB[si], self.pbB[bu]], w=[ACTB])
                for dt_ in range(8):
                    wt, wb = self.load_w(self.wd_s[li][:, dt_, :, :], "d")
                    bi = self.nxt_bank()
                    ps = self.pb[bi]
                    for fk in range(NFK):
                        self.E(self.pe, lambda h, fk=fk, wt=wt, ps=ps: h.matmul(ps[:], wt[:, fk, :], ACT_T[:, fk, :], start=(fk == 0), stop=(fk == NFK - 1)),
                               r=[wb, ACTB], w=[self.pbB[bi]])
                    self.E(self.dve, lambda h, dt_=dt_, ps=ps: h.tensor_tensor(out=XS[:, dt_, :], in0=XS[:, dt_, :], in1=ps[:], op=ALU.add),
                           r=[self.pbB[bi], XSB], w=[XSB])
                if last and self.do_final:
                    self.norm(XS[:], XSB, self.gFin[:], OF[:], OFB, SQv, ACTB, RS[:], RSB)
                    self.dma(odst[:, :, t0:t0 + 512], OF[:], [OFB], [self.oB], "ost")
                else:
                    self.dma(xdst[:, :, t0:t0 + 512], XS[:], [XSB], [self.xB[sq][j]], "xst")
            self.barrier()

    def emit(self):
        nc = self.nc
        with nc.Block() as block:
            def run(res):
                def f(h):
                    for op in res.ops:
                        if op[0] == "w":
                            h.wait_ge(op[1], op[2])
                        else:
                            op[1](h).then_inc(op[2], op[3])
                return f
            block.tensor(run(self.pe))
            block.scalar(run(self.act))
            block.vector(run(self.dve))
            block.gpsimd(run(self.pool))
            block.sync(run(self.sp))


def make_program(S, layers, nseq, do_final):
    b = Builder(S, layers, nseq, do_final)
    b.xB = [b.bufs(S // 512, "x") for _ in range(nseq)]
    b.oB = b.buf("out")
    nc = b.build()
    b.es.close()
    return nc


def rel_bucket_np(dist):
    n = np.maximum(dist, 0)
    nf = np.maximum(n, 1).astype(np.float32)
    large = 16 + (np.log(nf / np.float32(16)) / np.float32(math.log(128 / 16)) * np.float32(16)).astype(np.int32)
    large = np.minimum(large, 31)
    return np.where(n < 16, n, large)


def host_consts(rel_bias, layers):
    ss = np.arange(128)[:, None]
    v = np.arange(GW)[None, :]
    dist = v - ss
    idx = rel_bucket_np(dist)
    gt = np.transpose(rel_bias[idx], (2, 0, 1)).astype(np.float32)
    gt = np.where(dist[None] >= 0, gt, np.float32(NEG)).astype(np.float32)
    cb31 = np.broadcast_to(rel_bias[31][None, :], (128, 12)).astype(np.float32).copy()
    ident = np.eye(128, dtype=np.float32)
    tt = np.arange(128)[:, None]
    s2 = np.arange(128)[None, :]
    causneg = np.where(s2 <= tt, 0.0, NEG).astype(np.float32)
    lconst = np.zeros((len(layers), 128, 2), np.float32)
    for i, l in enumerate(layers):
        li_ = 0.8 - 0.6 * math.exp(-0.3 * l)
        lconst[i, :, 0] = 1.0 - li_
        lconst[i, :, 1] = li_
    return dict(gtab=np.ascontiguousarray(gt), cb31=cb31, ident=ident, causneg=causneg, lconst=lconst)


def make_in_maps(inp, layers, seq_groups, S):
    L = len(layers)
    ls = list(layers)
    c = host_consts(np.asarray(inp["rel_bias"], np.float32), ls)
    rep = lambda a: np.ascontiguousarray(np.broadcast_to(a[:, None, :], (a.shape[0], 128, a.shape[1])))
    common = dict(
        w_in=np.ascontiguousarray(inp["w_in"][ls]), w_out=np.ascontiguousarray(inp["w_out"][ls]),
        w_gate=np.ascontiguousarray(inp["w_gate"][ls]), w_up=np.ascontiguousarray(inp["w_up"][ls]),
        w_down=np.ascontiguousarray(inp["w_down"][ls]),
        gattn=np.ascontiguousarray(inp["attn_norm"][ls].reshape(L, 8, 128).transpose(0, 2, 1)),
        gffn=np.ascontiguousarray(inp["ffn_norm"][ls].reshape(L, 8, 128).transpose(0, 2, 1)),
        gfin=np.ascontiguousarray(inp["final_norm"].reshape(8, 128).T),
        lamrep=rep(inp["diff_lambda"][ls].reshape(L, 256)),
        sublnrep=rep(inp["diff_subln"][ls]),
        **c)
    maps = []
    for g in seq_groups:
        m = dict(common)
        m["xT"] = np.ascontiguousarray(np.transpose(inp["x"][g], (0, 2, 1)))
        maps.append(m)
    return maps


_PROG = {}


def kernel(**inputs):
    inp = {k: np.asarray(v, dtype=np.float32) for k, v in inputs.items()}
    B, S, _ = inp["x"].shape
    ncore = 8
    per = B // ncore
    layers = list(range(DEPTH))
    key = (S, tuple(layers), per)
    if key not in _PROG:
        _PROG[key] = make_program(S, layers, per, True)
    nc = _PROG[key]
    groups = [list(range(c * per, (c + 1) * per)) for c in range(ncore)]
    maps = make_in_maps(inp, layers, groups, S)
    res = run_bass_kernel_spmd(nc, maps, core_ids=list(range(ncore)))
    out = np.empty((B, S, D), np.float32)
    for c in range(ncore):
        o = res.results[c]["outT"]
        for i, b in enumerate(groups[c]):
            out[b] = o[i].T
    return out
```

```python
import math
from contextlib import ExitStack
import numpy as np
import concourse.bass as bass
import concourse.mybir as mybir
from concourse.bass_utils import run_bass_kernel_spmd

F32 = mybir.dt.float32
BF16 = mybir.dt.bfloat16
AF = mybir.ActivationFunctionType
ALU = mybir.AluOpType
AX = mybir.AxisListType

D = 1024
DEPTH = 4
DFF = 2816
NFK = DFF // 128
INC = 3656
EPS = 1e-6
NEG = -30000.0
GW = 640
W0 = 1024.0
NB = 24
O_DQ, O_DK, O_DV, O_IQ, O_IK, O_IW, O_FQ, O_FK, O_FV = 0, 512, 1024, 1536, 2048, 2112, 2120, 2632, 3144


class Buf:
    __slots__ = ("name", "lw", "rd", "excl")

    def __init__(self, name, excl=False):
        self.name = name
        self.lw = None
        self.rd = {}
        self.excl = excl


class Res:
    def __init__(self, name, sem, step, handle=None):
        self.name, self.sem, self.step, self.h = name, sem, step, handle
        self.count = 0
        self.known = {}
        self.ops = []


class Builder:
    def __init__(self, S, layers, nseq, do_final, x_from_input=True):
        self.S, self.layers, self.nseq, self.do_final = S, layers, nseq, do_final
        self.NCH = S // 512
        self.NT = S // 128
        self.KSEL = min(256, S // 4)
        self.nc = bass.Bass("TRN2", target_bir_lowering=False)
        self.es = ExitStack()
        self.chans = {}
        self.nbuf = 0

    def sem(self, name):
        return self.es.enter_context(self.nc.semaphore(name))

    def chan(self, name):
        if name not in self.chans:
            self.chans[name] = Res(name, self.sem("c_" + name), 16)
        return self.chans[name]

    def buf(self, name="b"):
        self.nbuf += 1
        return Buf(f"{name}{self.nbuf}")

    def bufs(self, n, name="b"):
        return [self.buf(name) for _ in range(n)]

    def _wait(self, eng, deps):
        for (res, val) in deps:
            if eng.known.get(res, 0) >= val:
                continue
            eng.known[res] = val
            eng.ops.append(("w", res.sem, val * res.step))

    def _deps(self, eng, r, w, is_dma=False):
        deps = []
        for b in r:
            if b.lw is not None:
                deps.append(b.lw)
            if b.excl:
                for res, v in b.rd.items():
                    if res is not eng:
                        deps.append((res, v))
        for b in w:
            if b.lw is not None:
                deps.append(b.lw)
            for res, v in b.rd.items():
                deps.append((res, v))
        out = []
        for (res, v) in deps:
            if res is eng and (eng.name == "pe"):
                continue
            out.append((res, v))
        return out

    def E(self, eng, fn, r=(), w=()):
        deps = self._deps(eng, r, w)
        deps2 = []
        for (res, v) in deps:
            deps2.append((res, v))
        self._wait(eng, deps2)
        eng.count += 1
        eng.ops.append(("i", fn, eng.sem, 1))
        me = (eng, eng.count)
        for b in r:
            b.rd[eng] = eng.count
        for b in w:
            b.lw = me
            b.rd = {}

    def dma(self, out, in_, r, w, chan, q=None):
        ch = self.chan(chan)
        q = q or self.sp
        deps = self._deps(q, r, w)
        if ch.count > 0:
            deps.append((ch, ch.count))
        self._wait(q, deps)
        ch.count += 1
        q.ops.append(("i", (lambda h, o=out, i=in_: h.dma_start(out=o, in_=i)), ch.sem, 16))
        me = (ch, ch.count)
        for b in r:
            b.rd[ch] = ch.count
        for b in w:
            b.lw = me
            b.rd = {}

    def barrier(self):
        allres = self.engs + list(self.chans.values())
        for e in self.engs:
            self._wait(e, [(x, x.count) for x in allres if x is not e and x.count > 0])

    def sb(self, name, shape, dt, stack=None):
        self.nbuf += 1
        return (stack or self.es).enter_context(self.nc.sbuf_tensor(f"{name}_{self.nbuf}", shape, dt))

    def evac(self, idx, out, in_, r, w, scale=None):
        if idx % 2 == 0:
            self.E(self.act, lambda h: h.activation(out=out, in_=in_, func=AF.Copy), r=r, w=w)
        else:
            self.E(self.dve, lambda h: h.tensor_copy(out=out, in_=in_), r=r, w=w)

    def build(self):
        nc, S, NCH, NT = self.nc, self.S, self.NCH, self.NT
        L, NSEQ = len(self.layers), self.nseq
        es = self.es
        dt_in = lambda name, shape, dt=F32: nc.dram_tensor(name, shape, dt, kind="ExternalInput").ap()
        self.xT = dt_in("xT", [NSEQ, D, S])
        self.w_in = dt_in("w_in", [L, D, INC])
        self.w_out = dt_in("w_out", [L, D, D])
        self.w_gate = dt_in("w_gate", [L, D, DFF])
        self.w_up = dt_in("w_up", [L, D, DFF])
        self.w_down = dt_in("w_down", [L, DFF, D])
        self.gattn = dt_in("gattn", [L, 128, 8])
        self.gffn = dt_in("gffn", [L, 128, 8])
        self.gfin = dt_in("gfin", [128, 8])
        self.lamrep = dt_in("lamrep", [L, 128, 256])
        self.sublnrep = dt_in("sublnrep", [L, 128, 128])
        self.lconst = dt_in("lconst", [L, 128, 2])
        self.gtab = dt_in("gtab", [12, 128, GW])
        self.cb31 = dt_in("cb31", [128, 12])
        self.ident_in = dt_in("ident", [128, 128])
        self.causneg_in = dt_in("causneg", [128, 128])
        self.outT = nc.dram_tensor("outT", [NSEQ, D, S], F32, kind="ExternalOutput").ap()
        self.xs = nc.dram_tensor("xs", [NSEQ, D, S], F32).ap()
        self.win_fm = nc.dram_tensor("win_fm", [L, 128, 21, 8, 128], BF16).ap()
        self.win_v = nc.dram_tensor("win_v", [L, 128, 2, 8, 512], BF16).ap()
        self.win_iw = nc.dram_tensor("win_iw", [L, 128, 8, 8], BF16).ap()
        self.wo_s = nc.dram_tensor("wo_s", [L, 128, 8, 8, 128], BF16).ap()
        self.wg_s = nc.dram_tensor("wg_s", [L, 128, NFK, 8, 128], BF16).ap()
        self.wu_s = nc.dram_tensor("wu_s", [L, 128, NFK, 8, 128], BF16).ap()
        self.wd_s = nc.dram_tensor("wd_s", [L, 128, 8, NFK, 128], BF16).ap()
        self.dsaT_d = nc.dram_tensor("dsaT_d", [128, 4, S], BF16).ap()
        self.maskT_d = nc.dram_tensor("maskT_d", [NT, 128, 512], BF16).ap()

        mk = lambda name, h: Res(name, self.sem("s_" + name), 1, h)
        self.pe = mk("pe", nc.tensor)
        self.act = mk("act", nc.scalar)
        self.dve = mk("dve", nc.vector)
        self.pool = mk("pool", nc.gpsimd)
        self.sp = mk("sp", nc.sync)
        self.engs = [self.pe, self.act, self.dve, self.pool, self.sp]

        self.pb = [es.enter_context(nc.psum_tensor(f"pb{i}", [128, 512], F32)) for i in range(6)]
        self.pbB = [Buf(f"pb{i}", excl=True) for i in range(6)]
        self.ptp = [es.enter_context(nc.psum_tensor(f"ptp{i}", [128, 1024], BF16)) for i in range(2)]
        self.ptpB = [Buf(f"ptp{i}", excl=True) for i in range(2)]
        self.tpi = 0

        self.ident = self.sb("ident", [128, 128], BF16)
        self.ones = self.sb("ones", [128, 128], BF16)
        self.causneg = self.sb("causneg", [128, 128], F32)
        self.cb = self.sb("cb", [128, 12], F32)
        self.epsT = self.sb("epsT", [128, 1], F32)
        self.gA = self.sb("gA", [128, L, 8], F32)
        self.gF = self.sb("gF", [128, L, 8], F32)
        self.gFin = self.sb("gFin", [128, 8], F32)
        self.lam = self.sb("lam", [128, L, 256], F32)
        self.subln = self.sb("subln", [128, L, 128], F32)
        self.lc = self.sb("lc", [128, L, 2], F32)
        self.lamneg = self.sb("lamneg", [128, L], F32)
        self.CONST = self.buf("const")
        stage = self.sb("cstage", [128, 128], F32)
        C = self.CONST
        self.dma(stage[:], self.ident_in, [], [C], "c0")
        self.E(self.dve, lambda h: h.tensor_copy(out=self.ident[:], in_=stage[:]), r=[C], w=[C])
        self.E(self.dve, lambda h: h.memset(self.ones[:], 1.0), w=[C])
        self.E(self.dve, lambda h: h.memset(self.epsT[:], EPS), w=[C])
        self.dma(self.causneg[:], self.causneg_in, [], [C], "c0")
        self.dma(self.cb[:], self.cb31, [], [C], "c0")
        self.dma(self.gFin[:], self.gfin, [], [C], "c0")
        for l in range(L):
            self.dma(self.gA[:, l, :], self.gattn[l], [], [C], "c0")
            self.dma(self.gF[:, l, :], self.gffn[l], [], [C], "c0")
            self.dma(self.lam[:, l, :], self.lamrep[l], [], [C], "c0")
            self.dma(self.subln[:, l, :], self.sublnrep[l], [], [C], "c0")
            self.dma(self.lc[:, l, :], self.lconst[l], [], [C], "c0")
        lp = self.sb("lp", [128, 2, 64], F32)
        ls = self.sb("ls", [128, 2], F32)
        for l in range(L):
            lv = self.lam[:, l, :].rearrange("p (a b d) -> p a b d", a=2, b=2)
            self.E(self.dve, lambda h, lv=lv: h.tensor_tensor(out=lp[:], in0=lv[:, :, 0, :], in1=lv[:, :, 1, :], op=ALU.mult), r=[C], w=[C])
            self.E(self.dve, lambda h: h.tensor_reduce(out=ls[:], in_=lp[:], axis=AX.X, op=ALU.add), r=[C], w=[C])
            self.E(self.act, lambda h: h.activation(out=ls[:], in_=ls[:], func=AF.Exp), r=[C], w=[C])
            self.E(self.dve, lambda h, l=l: h.tensor_tensor(out=self.lamneg[:, l:l + 1], in0=ls[:, 1:2], in1=ls[:, 0:1], op=ALU.subtract), r=[C], w=[C])
            self.E(self.dve, lambda h, l=l: h.tensor_tensor(out=self.lamneg[:, l:l + 1], in0=self.lamneg[:, l:l + 1], in1=self.lc[:, l, 1:2], op=ALU.subtract), r=[C], w=[C])
        self.barrier()

        for li, l in enumerate(self.layers):
            if li == 0:
                self.phase0(li)
            self.bg_items = self.p0_items(li + 1) if li + 1 < L else []
            self.bg_total = len(self.bg_items)
            self.bg_slices = NSEQ * NCH
            self.bg_done = 0
            for sq in range(NSEQ):
                src = self.xT if li == 0 else self.xs
                self.phase1(li, sq, src)
                self.phase2(li, sq, src, last=(li == L - 1))
        self.barrier()
        self.emit()
        return nc

    PW = 1096

    def p0_items(self, li):
        items = []
        fm = self.win_fm[li]
        for rc in range(8):
            rows = slice(rc * 128, (rc + 1) * 128)
            items.append((self.w_in[li, rows, 0:1024], 1024, [(fm[:, 0:8, rc, :], 0, 1024, 128)]))
            items.append((self.w_in[li, rows, 1024:2120], 1096, [
                (self.win_v[li][:, 0, rc, :], 0, 512, None),
                (fm[:, 8:12, rc, :], 512, 1024, 128),
                (fm[:, 12, rc, 0:64], 1024, 1088, None),
                (fm[:, 12, rc, 64:128], 1024, 1088, None),
                (self.win_iw[li][:, rc, :], 1088, 1096, None)]))
            items.append((self.w_in[li, rows, 2120:3144], 1024, [(fm[:, 13:21, rc, :], 0, 1024, 128)]))
            items.append((self.w_in[li, rows, 3144:3656], 512, [(self.win_v[li][:, 1, rc, :], 0, 512, None)]))
            for (wsrc, wdst) in ((self.w_gate, self.wg_s), (self.w_up, self.wu_s)):
                for c0 in (0, 1024, 2048):
                    c1 = min(DFF, c0 + 1024)
                    items.append((wsrc[li, rows, c0:c1], c1 - c0, [(wdst[li][:, c0 // 128:c1 // 128, rc, :], 0, c1 - c0, 128)]))
            items.append((self.w_out[li, rows, :], D, [(self.wo_s[li][:, :, rc, :], 0, D, 128)]))
        for fk in range(NFK):
            rows = slice(fk * 128, (fk + 1) * 128)
            items.append((self.w_down[li, rows, :], D, [(self.wd_s[li][:, :, fk, :], 0, D, 128)]))
        return items

    def p0_emit(self, item, st, bg):
        src, ncols, stores = item
        i = st["k"] % 2
        st["k"] += 1
        stg, cst, sB, cB = st["stg"], st["cst"], st["sB"], st["cB"]
        tag = "b" if bg else "f"
        q = self.pool if bg else self.sp
        self.dma(stg[i][:, 0:ncols], src, [], [sB[i]], f"p0l{tag}{i}", q=q)
        if bg:
            self.E(self.pool, lambda h: h.tensor_copy(out=cst[i][:, 0:ncols], in_=stg[i][:, 0:ncols]), r=[sB[i]], w=[cB[i]])
        elif i == 0:
            self.E(self.dve, lambda h: h.tensor_copy(out=cst[i][:, 0:ncols], in_=stg[i][:, 0:ncols]), r=[sB[i]], w=[cB[i]])
        else:
            self.E(self.act, lambda h: h.activation(out=cst[i][:, 0:ncols], in_=stg[i][:, 0:ncols], func=AF.Copy), r=[sB[i]], w=[cB[i]])
        for si, (dst, c0, c1, wd) in enumerate(stores):
            srcv = cst[i][:, c0:c1]
            if wd is not None:
                srcv = srcv.rearrange("p (n c) -> p n c", c=wd)
            self.dma(dst, srcv, [cB[i]], [self.buf("wst")], f"p0s{tag}{i}_{si}", q=q)

    def p0_state(self, stack):
        return dict(k=0, stg=[self.sb(f"p0s{i}", [128, self.PW], F32, stack) for i in range(2)],
                    cst=[self.sb(f"p0c{i}", [128, self.PW], BF16, stack) for i in range(2)],
                    sB=self.bufs(2, "p0s"), cB=self.bufs(2, "p0c"))

    def phase0(self, li):
        self.WS = self.buf("wscr")
        with ExitStack() as st:
            state = self.p0_state(st)
            for it in self.p0_items(li):
                self.p0_emit(it, state, False)
            self.barrier()

    def norm(self, X, XB, g, H, HB, SQ, SQB, RS, RSB, out_f32=False):
        self.E(self.act, lambda h: h.activation(out=SQ, in_=X, func=AF.Square), r=[XB], w=[SQB])
        bi = self.nxt_bank()
        ps = self.pb[bi]
        for dk in range(8):
            self.E(self.pe, lambda h, dk=dk: h.matmul(ps[:], self.ones[:], SQ[:, dk, :], start=(dk == 0), stop=(dk == 7)),
                   r=[SQB, self.CONST], w=[self.pbB[bi]])
        self.E(self.act, lambda h: h.activation(out=RS, in_=ps[:], func=AF.Sqrt, bias=self.epsT[:], scale=1.0 / D),
               r=[self.pbB[bi], self.CONST], w=[RSB])
        self.E(self.dve, lambda h: h.reciprocal(out=RS, in_=RS), r=[RSB], w=[RSB])
        for dk in range(8):
            self.E(self.dve, lambda h, dk=dk: h.scalar_tensor_tensor(out=H[:, dk, :], in0=X[:, dk, :], scalar=g[:, dk:dk + 1], in1=RS,
                                                                      op0=ALU.mult, op1=ALU.mult),
                   r=[XB, RSB, self.CONST], w=[HB])

    def nxt_bank(self):
        self.bank_i = (getattr(self, "bank_i", -1) + 1) % len(self.mmbanks)
        return self.mmbanks[self.bank_i]

    def load_w(self, src, shape_key):
        slots = self.wslots[shape_key]
        i = slots["i"] % len(slots["t"])
        slots["i"] += 1
        t, b = slots["t"][i], slots["b"][i]
        self.dma(t[:], src, [self.WS], [b], f"w{shape_key}{i}")
        return t, b

    def proj_fm(self, li, tiles, H, HB, dests):
        for n, (ti, (dst, dB)) in enumerate(zip(tiles, dests)):
            wt, wb = self.load_w(self.win_fm[li][:, ti, :, :], "fm")
            bi = self.nxt_bank()
            ps = self.pb[bi]
            for dk in range(8):
                self.E(self.pe, lambda h, dk=dk, wt=wt, ps=ps: h.matmul(ps[:], wt[:, dk, :], H[:, dk, :], start=(dk == 0), stop=(dk == 7)),
                       r=[wb, HB], w=[self.pbB[bi]])
            self.evac(n, dst, ps[:], [self.pbB[bi]], [dB])

    def attention(self, j, groups, nhg, vw, use_mask, G, GB, li, P1):
        pass

    def phase1(self, li, sq, src):
        nc, S, NCH, NT = self.nc, self.S, self.NCH, self.NT
        with ExitStack() as st:
            kT = self.sb("kT", [128, 4, S], BF16, st)
            vA = self.sb("vA", [128, NT, 8, 65], BF16, st)
            kiT = self.sb("kiT", [128, S], BF16, st)
            XS = self.sb("XS", [128, 8 * 512], F32, st)
            SQM = self.sb("SQM", [128, 8 * 512], BF16, st)
            hT = self.sb("hT", [128, 8, 512], BF16, st)
            qT = self.sb("qT", [128, 4, 512], BF16, st)
            iqT = self.sb("iqT", [128, 4, 512], BF16, st)
            Gt = self.sb("Gt", [128, 4, GW], F32, st)
            RS = self.sb("RS", [128, 512], F32, st)
            wsb = self.sb("wsb", [128, 4, 8], F32, st)
            Rt = [self.sb(f"Rt{i}", [128, 512], BF16, st) for i in range(4)]
            Pt = [self.sb(f"Pt{i}", [128, 512], BF16, st) for i in range(4)]
            Pm = [self.sb(f"Pm{i}", [128, 512], BF16, st) for i in range(4)]
            Tm = [self.sb(f"Tm{i}", [128, 512], F32, st) for i in range(2)]
            mst = [self.sb(f"mst{i}", [128, 4, 128], BF16, st) for i in range(2)]
            mT = [self.sb(f"mT{i}", [128, 512], BF16, st) for i in range(2)]
            osb = self.sb("osb", [128, 4, 512], BF16, st)
            dsT = self.sb("dsT", [128, 4, 512], BF16, st)
            rc = self.sb("rc", [128, 4], F32, st)
            bis = self.sb("bis", [128, 8], F32, st)
            XS_b = self.sb("XSb", [128, 8 * 512], F32, st)
            SQM_b = self.sb("SQMb", [128, 8 * 512], BF16, st)
            bis_b = self.sb("bisb", [128, 8], F32, st)
            dgs = [[self.sb(f"dg{q}{i}", [128, 128], BF16, st) for i in range(4)] for q in range(2)]
            wfm = [self.sb(f"wfm{i}", [128, 8, 128], BF16, st) for i in range(3)]
            wv = [self.sb(f"wv{i}", [128, 8, 512], BF16, st) for i in range(1)]
            wiw = [self.sb(f"wiw{i}", [128, 8, 8], BF16, st) for i in range(1)]
            self.wslots = {"fm": {"t": wfm, "b": self.bufs(3), "i": 0}, "v": {"t": wv, "b": self.bufs(1), "i": 0},
                           "iw": {"t": wiw, "b": self.bufs(1), "i": 0}}
            B = lambda n="p1": self.buf(n)
            kTB, vAB, kiTB = self.bufs(NCH), self.bufs(NCH), self.bufs(NCH)
            XSB, SQMB, hTB, qTB, iqTB, GB, RSB, wsbB = B(), B(), B(), B(), B(), B(), B(), B()
            RtB, PtB, PmB, TmB, mstB, mTB = self.bufs(4), self.bufs(4), self.bufs(4), self.bufs(2), self.bufs(2), self.bufs(2)
            osbB, dsTB, rcB, bisB = B(), B(), B(), B()
            XSs, SQMs, biss = [XS, XS_b], [SQM, SQM_b], [bis, bis_b]
            XSBs, SQMBs, SQABs = [XSB, B()], [SQMB, B()], [B(), B()]
            midBs, cntBs, sgBs, tmpBs, dgBs = self.bufs(2), self.bufs(2), self.bufs(2), self.bufs(2), self.bufs(2)
            mdB = self.buf("maskT_d")
            dsdB = self.dsdB = self.bufs(NCH, "dsaTd")
            Xv = XS[:].rearrange("p (k t) -> p k t", k=8)
            SQv = SQM[:].rearrange("p (k t) -> p k t", k=8)
            self.E(self.pool, lambda h: h.memset(vA[:, :, :, 64:65], 1.0), w=vAB)
            cnt, tmp, mid, theta, sg = bis[:, 0:1], bis[:, 1:2], bis[:, 2:3], bis[:, 3:4], bis[:, 4:5]
            xsrc = src[sq].rearrange("(k p) t -> p k t", p=128)
            for j in range(NCH):
                t0 = 512 * j
                self.mmbanks = [0, 1, 2, 3, 4, 5]
                self.dma(Xv, xsrc[:, :, t0:t0 + 512], [self.xB[sq][j]], [XSB], "x")
                self.norm(Xv, XSB, self.gA[:, li, :], hT[:], hTB, SQv, SQMB, RS[:], RSB)
                dests = [(qT[:, c, :], qTB) for c in range(4)] + [(kT[:, c, t0:t0 + 512], kTB[j]) for c in range(4)] + \
                        [(iqT[:, c, :], iqTB) for c in range(4)] + [(kiT[:, t0:t0 + 512], kiTB[j])]
                self.proj_fm(li, list(range(13)), hT, hTB, dests)
                wt, wb = self.load_w(self.win_v[li][:, 0, :, :], "v")
                wi, wib = self.load_w(self.win_iw[li], "iw")
                for tt in range(4):
                    bi = self.nxt_bank()
                    ps = self.pb[bi]
                    for dk in range(8):
                        self.E(self.pe, lambda h, dk=dk, tt=tt, ps=ps, wt=wt: h.matmul(ps[:], hT[:, dk, tt * 128:(tt + 1) * 128], wt[:, dk, :],
                                                                              start=(dk == 0), stop=(dk == 7)), r=[wb, hTB], w=[self.pbB[bi]])
                    self.evac(tt, vA[:, 4 * j + tt, :, 0:64], ps[:].rearrange("p (h e) -> p h e", h=8), [self.pbB[bi]], [vAB[j]])
                    bi = self.nxt_bank()
                    ps2 = self.pb[bi]
                    for dk in range(8):
                        self.E(self.pe, lambda h, dk=dk, tt=tt, ps2=ps2, wi=wi: h.matmul(ps2[:, 0:8], hT[:, dk, tt * 128:(tt + 1) * 128], wi[:, dk, :],
                                                                                start=(dk == 0), stop=(dk == 7)), r=[wib, hTB], w=[self.pbB[bi]])
                    self.E(self.dve, lambda h, tt=tt, ps2=ps2: h.tensor_copy(out=wsb[:, tt, :], in_=ps2[:, 0:8]), r=[self.pbB[bi]], w=[wsbB])
                for pair in ((0, 1), (2, 3)):
                    for sl, qi in enumerate(pair):
                        i = 4 * j + qi
                        XSc, XSBc = XSs[sl], XSBs[sl]
                        dgc, dgBc = dgs[sl], dgBs[sl]
                        for a in range(4):
                            self.E(self.dve, lambda h, a=a, qi=qi, dgc=dgc: h.tensor_scalar(out=dgc[a][:], in0=self.ident[:], scalar1=wsb[:, qi, 2 * a + 1:2 * a + 2], scalar2=None, op0=ALU.mult),
                                   r=[self.CONST, wsbB], w=[dgBc])
                        ib = [(kb, hd) for kb in range(j + 1) for hd in range(8)]
                        ILAG = 2

                        def ifront(n, qi=qi, j=j, ib=ib):
                            kb, hd = ib[n]
                            wdt = 512 if kb < j else 128 * (qi + 1)
                            pr = slice((hd % 2) * 64, (hd % 2) * 64 + 64)
                            db = (0, 1, 2, 5)[n % 4]
                            dps = self.pb[db]
                            self.E(self.pe, lambda h, pr=pr, hd=hd, dps=dps, kb=kb, wdt=wdt, qi=qi: h.matmul(
                                dps[:, 0:wdt], iqT[pr, hd // 2, qi * 128:(qi + 1) * 128], kiT[pr, kb * 512:kb * 512 + wdt], start=True, stop=True),
                                r=[iqTB] + kiTB[:j + 1], w=[self.pbB[db]])
                            ri = n % 4
                            if hd % 2 == 0:
                                self.E(self.dve, lambda h, dps=dps, ri=ri, wdt=wdt, qi=qi, hd=hd: h.tensor_scalar(
                                    out=Rt[ri][:, 0:wdt], in0=dps[:, 0:wdt], scalar1=0.0, scalar2=wsb[:, qi, hd:hd + 1], op0=ALU.max, op1=ALU.mult),
                                    r=[self.pbB[db], wsbB], w=[RtB[ri]])
                            else:
                                self.E(self.act, lambda h, dps=dps, ri=ri, wdt=wdt: h.activation(out=Rt[ri][:, 0:wdt], in_=dps[:, 0:wdt], func=AF.Relu),
                                       r=[self.pbB[db]], w=[RtB[ri]])

                        def iback(n, qi=qi, j=j, ib=ib, XSc=XSc, XSBc=XSBc, dgc=dgc, dgBc=dgBc):
                            kb, hd = ib[n]
                            wdt = 512 if kb < j else 128 * (qi + 1)
                            sbank = 3 + (kb % 2)
                            sps = self.pb[sbank]
                            ri = n % 4
                            if hd % 2 == 0:
                                self.E(self.pe, lambda h, sps=sps, ri=ri, wdt=wdt, hd=hd: h.matmul(
                                    sps[:, 0:wdt], self.ident[:], Rt[ri][:, 0:wdt], start=(hd == 0), stop=(hd == 7)),
                                    r=[RtB[ri], self.CONST], w=[self.pbB[sbank]])
                            else:
                                self.E(self.pe, lambda h, sps=sps, ri=ri, wdt=wdt, hd=hd: h.matmul(
                                    sps[:, 0:wdt], dgc[hd // 2][:], Rt[ri][:, 0:wdt], start=(hd == 0), stop=(hd == 7)),
                                    r=[RtB[ri], dgBc], w=[self.pbB[sbank]])
                            if hd == 7:
                                self.E(self.act, lambda h, sps=sps, kb=kb, wdt=wdt: h.activation(out=XSc[:, kb * 512:kb * 512 + wdt], in_=sps[:, 0:wdt], func=AF.Copy),
                                       r=[self.pbB[sbank]], w=[XSBc])

                        for n in range(0, len(ib) + ILAG, 2):
                            for m in (n, n + 1):
                                if m < len(ib):
                                    ifront(m)
                            for m in (n, n + 1):
                                if ILAG <= m < len(ib) + ILAG:
                                    iback(m - ILAG)
                        self.E(self.dve, lambda h, i=i, XSc=XSc: h.tensor_tensor(out=XSc[:, i * 128:(i + 1) * 128], in0=XSc[:, i * 128:(i + 1) * 128], in1=self.causneg[:], op=ALU.add),
                               r=[XSBc, self.CONST], w=[XSBc])
                    tiles = []
                    for sl, qi in enumerate(pair):
                        i = 4 * j + qi
                        nkeys = 128 * (i + 1)
                        bb = biss[sl]
                        tiles.append(dict(sl=sl, qi=qi, i=i, nkeys=nkeys, XS=XSs[sl], XSB=XSBs[sl], SQM=SQMs[sl], SQMB=SQMBs[sl], SQAB=SQABs[sl],
                                          cnt=bb[:, 0:1], tmp=bb[:, 1:2], mid=bb[:, 2:3], theta=bb[:, 3:4], sg=bb[:, 4:5],
                                          midB=midBs[sl], cntB=cntBs[sl], sgB=sgBs[sl], tmpB=tmpBs[sl],
                                          nD=max(128, int(round(0.44 * nkeys / 128)) * 128)))
                    for t in tiles:
                        if t["nkeys"] > self.KSEL:
                            self.E(self.dve, lambda h, t=t: h.memset(t["mid"], 0.0), w=[t["midB"]])
                        else:
                            self.E(self.dve, lambda h, t=t: h.memset(t["theta"], -W0), w=[t["midB"]])
                    for k in range(1, NB + 1):
                        c = W0 / 2 ** (k + 1) if k < NB else W0 / 2 ** NB
                        m2 = 2 * c if k < NB else c
                        for t in tiles:
                            if t["nkeys"] <= self.KSEL:
                                continue
                            nD, nkeys = t["nD"], t["nkeys"]
                            nA = nkeys - nD
                            dst = t["mid"] if k < NB else t["theta"]
                            self.E(self.dve, lambda h, t=t, nD=nD: h.tensor_scalar(out=t["SQM"][:, 0:nD], in0=t["XS"][:, 0:nD], scalar1=t["mid"], scalar2=None,
                                                                                   op0=ALU.is_ge, op1=ALU.add, accum_out=t["cnt"]), r=[t["XSB"], t["midB"]], w=[t["SQMB"], t["cntB"]])
                            self.E(self.act, lambda h, t=t, nD=nD, nkeys=nkeys: h.activation(out=t["SQM"][:, nD:nkeys], in_=t["XS"][:, nD:nkeys], func=AF.Sign, bias=t["mid"], scale=-1.0,
                                                                                             accum_out=t["sg"]), r=[t["XSB"], t["midB"]], w=[t["SQAB"], t["sgB"]])
                            self.E(self.dve, lambda h, t=t: h.scalar_tensor_tensor(out=t["tmp"], in0=t["sg"], scalar=-0.5, in1=t["cnt"], op0=ALU.mult, op1=ALU.add),
                                   r=[t["sgB"], t["cntB"]], w=[t["tmpB"]])
                            self.E(self.dve, lambda h, t=t, m2=m2, nA=nA: h.tensor_scalar(out=t["tmp"], in0=t["tmp"], scalar1=self.KSEL - 0.5 - nA / 2.0, scalar2=m2, op0=ALU.is_ge, op1=ALU.mult),
                                   r=[t["tmpB"]], w=[t["tmpB"]])
                            self.E(self.dve, lambda h, t=t, c=c, dst=dst: h.scalar_tensor_tensor(out=dst, in0=t["tmp"], scalar=-c, in1=t["mid"], op0=ALU.add, op1=ALU.add),
                                   r=[t["tmpB"], t["midB"]], w=[t["midB"]])
                    for t in tiles:
                        nkeys, qi, i = t["nkeys"], t["qi"], t["i"]
                        SQc, SQBc = t["SQM"], t["SQMB"]
                        self.E(self.dve, lambda h, t=t, nkeys=nkeys: h.tensor_scalar(out=t["SQM"][:, 0:nkeys], in0=t["XS"][:, 0:nkeys], scalar1=t["theta"], scalar2=None, op0=ALU.is_ge),
                               r=[t["XSB"], t["midB"]], w=[t["SQMB"], t["SQAB"]])
                        for g0 in range(0, i + 1, 4):
                            n = min(4, i + 1 - g0)
                            tp = self.tpi % 2
                            self.tpi += 1
                            for a in range(n):
                                stt = g0 + a
                                self.E(self.pe, lambda h, tp=tp, a=a, stt=stt, SQc=SQc: h.transpose(self.ptp[tp][:, a * 128:(a + 1) * 128], SQc[:, stt * 128:(stt + 1) * 128], self.ident[:]),
                                       r=[SQBc, self.CONST], w=[self.ptpB[tp]])
                            self.E(self.act, lambda h, tp=tp, n=n: h.activation(out=mst[tp][:, 0:n, :], in_=self.ptp[tp][:, 0:n * 128].rearrange("p (n c) -> p n c", c=128), func=AF.Copy),
                                   r=[self.ptpB[tp]], w=[mstB[tp]])
                            self.dma(self.maskT_d[g0:g0 + n, :, qi * 128:(qi + 1) * 128].rearrange("n s t -> s n t"), mst[tp][:, 0:n, :], [mstB[tp]], [mdB], f"mst{tp}")
                for hg in range(2):
                    self.dma(Gt[:], self.gtab[4 * hg:4 * hg + 4].rearrange("g p c -> p g c"), [], [GB], "G")
                    heads = []
                    for hh in range(4):
                        hd = 4 * hg + hh
                        pr = slice((hd % 2) * 64, (hd % 2) * 64 + 64)
                        heads.append(dict(
                            k=lambda s0, pr=pr, hd=hd: kT[pr, hd // 2, s0:s0 + 128],
                            q=lambda c0, pr=pr, hd=hd: qT[pr, hd // 2, c0:512],
                            gi=hh, cb=self.cb[:, hd:hd + 1],
                            v=lambda stt, hd=hd: vA[:, stt, hd, :], os=hh))
                    def fin(qi, hg=hg):
                        ops = self.pb[qi]
                        ov = ops[:, 0:260].rearrange("p (h e) -> p h e", e=65)
                        self.E(self.dve, lambda h: h.reciprocal(out=rc[:], in_=ov[:, :, 64]), r=[self.pbB[qi]], w=[rcB])
                        for hh in range(4):
                            hd = 4 * hg + hh
                            self.E(self.dve, lambda h, hh=hh, hd=hd: h.tensor_scalar(out=osb[:, qi, hd * 64:(hd + 1) * 64], in0=ov[:, hh, 0:64], scalar1=rc[:, hh:hh + 1],
                                                                                     scalar2=None, op0=ALU.mult), r=[self.pbB[qi], rcB], w=[osbB])
                        for ft in (2 * hg, 2 * hg + 1):
                            tp = self.tpi % 2
                            self.tpi += 1
                            self.E(self.pe, lambda h, tp=tp, ft=ft: h.transpose(self.ptp[tp][:, 0:128], osb[:, qi, ft * 128:(ft + 1) * 128], self.ident[:]),
                                   r=[osbB, self.CONST], w=[self.ptpB[tp]])
                            self.E(self.act, lambda h, tp=tp, ft=ft: h.activation(out=dsT[:, ft, qi * 128:(qi + 1) * 128], in_=self.ptp[tp][:, 0:128], func=AF.Copy),
                                   r=[self.ptpB[tp]], w=[dsTB])
                    self.attn_group(j, heads, 65, (mT, mTB, mdB), Gt, GB, Pt, PtB, Pm, PmB, Tm, TmB, kTB, vAB, qTB, fin)
                self.dma(self.dsaT_d[:, :, t0:t0 + 512], dsT[:], [dsTB], [dsdB[j]], "dsst")
            self.barrier()

    def attn_group(self, j, heads, vw1, mask, Gt, GB, Pt, PtB, Pm, PmB, Tm, TmB, kB, vB, qB, fin):
        nst = 4 * j + 4
        base = getattr(self, "_ac", 0)
        blocks = [(stt, hd) for stt in range(nst) for hd in heads]
        LAG = 2
        st_ = {}

        def front(n):
            stt, hd = blocks[n]
            g = base + n
            s0 = 128 * stt
            r = 4 * j - stt
            col0 = 0 if r >= 0 else -128 * r
            N = 512 - col0
            near = r <= 1
            mi = stt % 2
            if mask is not None and hd is heads[0]:
                mT, mTB, mdB = mask
                self.dma(mT[mi][:, col0:512], self.maskT_d[stt][:, col0:512], [mdB], [mTB[mi]], f"mT{mi}")
            sb = 4 + g % 2
            pi = g % 4
            sps = self.pb[sb]
            self.E(self.pe, lambda h, hd=hd, sps=sps, s0=s0, col0=col0, N=N: h.matmul(sps[:, 0:N], hd["k"](s0), hd["q"](col0), start=True, stop=True),
                   r=[qB] + kB[:j + 1], w=[self.pbB[sb]])
            if near:
                ti = g % 2
                g0 = 128 * r + col0
                self.E(self.dve, lambda h, hd=hd, sps=sps, ti=ti, N=N, g0=g0: h.scalar_tensor_tensor(
                    out=Tm[ti][:, 0:N], in0=sps[:, 0:N], scalar=0.125, in1=Gt[:, hd["gi"], g0:g0 + N], op0=ALU.mult, op1=ALU.add),
                    r=[self.pbB[sb], GB], w=[TmB[ti]])
                self.E(self.act, lambda h, ti=ti, pi=pi, N=N: h.activation(out=Pt[pi][:, 0:N], in_=Tm[ti][:, 0:N], func=AF.Exp),
                       r=[TmB[ti]], w=[PtB[pi]])
            else:
                self.E(self.act, lambda h, hd=hd, sps=sps, pi=pi, N=N: h.activation(out=Pt[pi][:, 0:N], in_=sps[:, 0:N], func=AF.Exp, bias=hd["cb"], scale=0.125),
                       r=[self.pbB[sb], self.CONST], w=[PtB[pi]])
            if mask is not None:
                mT, mTB, mdB = mask
                eng = self.pool if g % 3 == 0 else self.dve
                self.E(eng, lambda h, pi=pi, mi=mi, N=N, col0=col0, mT=mT: h.tensor_tensor(out=Pm[pi][:, 0:N], in0=Pt[pi][:, 0:N], in1=mT[mi][:, col0:512], op=ALU.mult),
                       r=[PtB[pi], mTB[mi]], w=[PmB[pi]])
                st_[n] = (Pm[pi], PmB[pi], col0)
            else:
                st_[n] = (Pt[pi], PtB[pi], col0)

        def back(n):
            stt, hd = blocks[n]
            PP, PPB, col0 = st_.pop(n)
            for qi in range(col0 // 128, 4):
                last = 4 * j + qi
                o = self.pb[qi][:, hd["os"] * vw1:(hd["os"] + 1) * vw1]
                self.E(self.pe, lambda h, o=o, PP=PP, qi=qi, col0=col0, hd=hd, stt=stt, last=last: h.matmul(
                    o, PP[:, qi * 128 - col0:qi * 128 - col0 + 128], hd["v"](stt), start=(stt == 0 and hd is heads[0]), stop=(stt == last and hd is heads[-1])),
                    r=[PPB] + vB[:j + 1], w=[self.pbB[qi]])

        for n in range(0, len(blocks) + LAG, 2):
            for m in (n, n + 1):
                if m < len(blocks):
                    front(m)
            for m in (n, n + 1):
                if LAG <= m < len(blocks) + LAG:
                    back(m - LAG)
        cnt = base + len(blocks)
        self._ac = cnt
        for qi in range(4):
            fin(qi)

    def phase2(self, li, sq, src, last):
        nc, S, NCH, NT = self.nc, self.S, self.NCH, self.NT
        with ExitStack() as st:
            kT = self.sb("fkT", [128, 4, S], BF16, st)
            vA = self.sb("fvA", [128, NT, 4, 129], BF16, st)
            XS = self.sb("XS2", [128, 8, 512], F32, st)
            ACT_T = self.sb("ACTT", [128, NFK, 512], BF16, st)
            hT = self.sb("hT2", [128, 8, 512], BF16, st)
            qT = self.sb("fqT", [128, 4, 512], BF16, st)
            Gt = self.sb("Gt2", [128, 4, GW], F32, st)
            RS = self.sb("RS2", [128, 512], F32, st)
            mix = self.sb("mix", [128, 8, 512], BF16, st)
            Pt = [self.sb(f"Pu{i}", [128, 512], BF16, st) for i in range(4)]
            Tm = [self.sb(f"Tn{i}", [128, 512], F32, st) for i in range(2)]
            osb = self.sb("osb2", [128, 4, 512], BF16, st)
            o1 = self.sb("o1", [128, 128], F32, st)
            dd = self.sb("dd", [128, 128], F32, st)
            jk = self.sb("jk", [128, 128], F32, st)
            sm = self.sb("sm", [128, 8], F32, st)
            sil = [self.sb(f"sil{i}", [128, 512], F32, st) for i in range(2)]
            OF = self.sb("OF", [128, 8, 512], F32, st) if (last and self.do_final) else None
            bgst = self.p0_state(st) if self.bg_items else None
            wfm = [self.sb(f"xfm{i}", [128, 8, 128], BF16, st) for i in range(4)]
            wv = [self.sb(f"xv{i}", [128, 8, 512], BF16, st) for i in range(1)]
            wd = [self.sb(f"xd{i}", [128, NFK, 128], BF16, st) for i in range(2)]
            self.wslots = {"fm": {"t": wfm, "b": self.bufs(4), "i": 0}, "v": {"t": wv, "b": self.bufs(1), "i": 0},
                           "d": {"t": wd, "b": self.bufs(2), "i": 0}}
            B = lambda n="p2": self.buf(n)
            kTB, vAB = self.bufs(NCH), self.bufs(NCH)
            XSB, ACTB, hTB, qTB, GB, RSB, mixB, osbB, smB, OFB = B(), B(), B(), B(), B(), B(), B(), B(), B(), B()
            PtB, TmB, silB = self.bufs(4), self.bufs(2), self.bufs(2)
            SQv = ACT_T[:, 0:8, :]
            self.E(self.pool, lambda h: h.memset(vA[:, :, :, 128:129], 1.0), w=vAB)
            self.dma(Gt[:], self.gtab[8:12].rearrange("g p c -> p g c"), [], [GB], "G")
            xsrc = src[sq].rearrange("(k p) t -> p k t", p=128)
            xdst = self.xs[sq].rearrange("(k p) t -> p k t", p=128)
            odst = self.outT[sq].rearrange("(k p) t -> p k t", p=128)
            r1, r2, ssq, rstd = sm[:, 0:1], sm[:, 1:2], sm[:, 2:3], sm[:, 3:4]
            for j in range(NCH):
                t0 = 512 * j
                self.mmbanks = [0, 1, 2, 3, 4, 5]
                self.dma(XS[:], xsrc[:, :, t0:t0 + 512], [self.xB[sq][j]], [XSB], "x")
                self.norm(XS[:], XSB, self.gA[:, li, :], hT[:], hTB, SQv, ACTB, RS[:], RSB)
                dests = [(qT[:, c, :], qTB) for c in range(4)] + [(kT[:, c, t0:t0 + 512], kTB[j]) for c in range(4)]
                self.proj_fm(li, list(range(13, 21)), hT, hTB, dests)
                wt, wb = self.load_w(self.win_v[li][:, 1, :, :], "v")
                for tt in range(4):
                    bi = self.nxt_bank()
                    ps = self.pb[bi]
                    for dk in range(8):
                        self.E(self.pe, lambda h, dk=dk, tt=tt, ps=ps, wt=wt: h.matmul(ps[:], hT[:, dk, tt * 128:(tt + 1) * 128], wt[:, dk, :],
                                                                              start=(dk == 0), stop=(dk == 7)), r=[wb, hTB], w=[self.pbB[bi]])
                    self.evac(tt, vA[:, 4 * j + tt, :, 0:128], ps[:].rearrange("p (h e) -> p h e", h=4), [self.pbB[bi]], [vAB[j]])
                self.dma(mix[:, 0:4, :], self.dsaT_d[:, :, t0:t0 + 512], [self.dsdB[j]], [mixB], "mixl")
                for hh in range(4):
                    heads = []
                    for c in range(2):
                        pr = slice(c * 64, c * 64 + 64)
                        heads.append(dict(
                            k=lambda s0, pr=pr, hh=hh: kT[pr, hh, s0:s0 + 128],
                            q=lambda c0, pr=pr, hh=hh: qT[pr, hh, c0:512],
                            gi=hh, cb=self.cb[:, 8 + hh:9 + hh],
                            v=lambda stt, hh=hh: vA[:, stt, hh, :], os=c))
                    def fin(qi, hh=hh):
                        ops = self.pb[qi]
                        ov = ops[:, 0:258].rearrange("p (c e) -> p c e", e=129)
                        self.E(self.dve, lambda h: h.reciprocal(out=sm[:, 0:2], in_=ov[:, :, 128]), r=[self.pbB[qi]], w=[smB])
                        self.E(self.dve, lambda h: h.tensor_tensor(out=r2, in0=r2, in1=self.lamneg[:, li:li + 1], op=ALU.mult), r=[smB, self.CONST], w=[smB])
                        self.E(self.dve, lambda h: h.tensor_scalar(out=o1[:], in0=ov[:, 0, 0:128], scalar1=r1, scalar2=None, op0=ALU.mult), r=[self.pbB[qi], smB], w=[smB])
                        self.E(self.dve, lambda h: h.scalar_tensor_tensor(out=dd[:], in0=ov[:, 1, 0:128], scalar=r2, in1=o1[:], op0=ALU.mult, op1=ALU.add),
                               r=[self.pbB[qi], smB], w=[smB])
                        self.E(self.act, lambda h: h.activation(out=jk[:], in_=dd[:], func=AF.Square, accum_out=ssq), r=[smB], w=[smB])
                        self.E(self.act, lambda h: h.activation(out=rstd, in_=ssq, func=AF.Sqrt, bias=self.epsT[:], scale=1.0 / 128), r=[smB, self.CONST], w=[smB])
                        self.E(self.dve, lambda h: h.reciprocal(out=rstd, in_=rstd), r=[smB], w=[smB])
                        self.E(self.dve, lambda h: h.tensor_tensor(out=rstd, in0=rstd, in1=self.lc[:, li, 0:1], op=ALU.mult), r=[smB, self.CONST], w=[smB])
                        self.E(self.dve, lambda h: h.scalar_tensor_tensor(out=osb[:, qi, hh * 128:(hh + 1) * 128], in0=dd[:], scalar=rstd, in1=self.subln[:, li, :],
                                                                          op0=ALU.mult, op1=ALU.mult), r=[smB, self.CONST], w=[osbB])
                        tp = self.tpi % 2
                        self.tpi += 1
                        self.E(self.pe, lambda h, tp=tp: h.transpose(self.ptp[tp][:, 0:128], osb[:, qi, hh * 128:(hh + 1) * 128], self.ident[:]),
                               r=[osbB, self.CONST], w=[self.ptpB[tp]])
                        self.E(self.act, lambda h, tp=tp: h.activation(out=mix[:, 4 + hh, qi * 128:(qi + 1) * 128], in_=self.ptp[tp][:, 0:128], func=AF.Copy),
                               r=[self.ptpB[tp]], w=[mixB])
                    self.attn_group(j, heads, 129, None, Gt, GB, Pt, PtB, None, None, Tm, TmB, kTB, vAB, qTB, fin)
                if self.bg_items:
                    self.bg_done += 1
                    upto = (self.bg_total * self.bg_done + self.bg_slices - 1) // self.bg_slices
                    nit = upto - (self.bg_total - len(self.bg_items))
                    for _ in range(max(0, nit)):
                        if self.bg_items:
                            self.p0_emit(self.bg_items.pop(0), bgst, True)
                for dt_ in range(8):
                    wt, wb = self.load_w(self.wo_s[li][:, dt_, :, :], "fm")
                    bi = self.nxt_bank()
                    ps = self.pb[bi]
                    for ck in range(8):
                        self.E(self.pe, lambda h, ck=ck, wt=wt, ps=ps: h.matmul(ps[:], wt[:, ck, :], mix[:, ck, :], start=(ck == 0), stop=(ck == 7)),
                               r=[wb, mixB], w=[self.pbB[bi]])
                    self.E(self.dve, lambda h, dt_=dt_, ps=ps: h.tensor_tensor(out=XS[:, dt_, :], in0=XS[:, dt_, :], in1=ps[:], op=ALU.add),
                           r=[self.pbB[bi], XSB], w=[XSB])
                self.norm(XS[:], XSB, self.gF[:, li, :], hT[:], hTB, SQv, ACTB, RS[:], RSB)
                for ft in range(NFK):
                    wg, wgb = self.load_w(self.wg_s[li][:, ft, :, :], "fm")
                    wu, wub = self.load_w(self.wu_s[li][:, ft, :, :], "fm")
                    bg = self.nxt_bank()
                    bu = self.nxt_bank()
                    pg, pu = self.pb[bg], self.pb[bu]
                    for dk in range(8):
                        self.E(self.pe, lambda h, dk=dk, wg=wg, pg=pg: h.matmul(pg[:], wg[:, dk, :], hT[:, dk, :], start=(dk == 0), stop=(dk == 7)),
                               r=[wgb, hTB], w=[self.pbB[bg]])
                    for dk in range(8):
                        self.E(self.pe, lambda h, dk=dk, wu=wu, pu=pu: h.matmul(pu[:], wu[:, dk, :], hT[:, dk, :], start=(dk == 0), stop=(dk == 7)),
                               r=[wub, hTB], w=[self.pbB[bu]])
                    si = ft % 2
                    self.E(self.act, lambda h, si=si, pg=pg: h.activation(out=sil[si][:], in_=pg[:], func=AF.Silu), r=[self.pbB[bg]], w=[silB[si]])
                    self.E(self.dve, lambda h, si=si, pu=pu, ft=ft: h.tensor_tensor(out=ACT_T[:, ft, :], in0=sil[si][:], in1=pu[:], op=ALU.mult),
                           r=[silB[si], self.pbB[bu]], w=[ACTB])
                for dt_ in range(8):
                    wt, wb = self.load_w(self.wd_s[li][:, dt_, :, :], "d")
                    bi = self.nxt_bank()
                    ps = self.pb[bi]
                    for fk in range(NFK):
                        self.E(self.pe, lambda h, fk=fk, wt=wt, ps=ps: h.matmul(ps[:], wt[:, fk, :], ACT_T[:, fk, :], start=(fk == 0), stop=(fk == NFK - 1)),
                               r=[wb, ACTB], w=[self.pbB[bi]])
                    self.E(self.dve, lambda h, dt_=dt_, ps=ps: h.tensor_tensor(out=XS[:, dt_, :], in0=XS[:, dt_, :], in1=ps[:], op=ALU.add),
                           r=[self.pbB[bi], XSB], w=[XSB])
                if last and self.do_final:
                    self.norm(XS[:], XSB, self.gFin[:], OF[:], OFB, SQv, ACTB, RS[:], RSB)
                    self.dma(odst[:, :, t0:t0 + 512], OF[:], [OFB], [self.oB], "ost")
                else:
                    self.dma(xdst[:, :, t0:t0 + 512], XS[:], [XSB], [self.xB[sq][j]], "xst")
            self.barrier()

    def emit(self):
        nc = self.nc
        with nc.Block() as block:
            def run(res):
                def f(h):
                    for op in res.ops:
                        if op[0] == "w":
                            h.wait_ge(op[1], op[2])
                        else:
                            op[1](h).then_inc(op[2], op[3])
                return f
            block.tensor(run(self.pe))
            block.scalar(run(self.act))
            block.vector(run(self.dve))
            block.gpsimd(run(self.pool))
            block.sync(run(self.sp))


def make_program(S, layers, nseq, do_final):
    b = Builder(S, layers, nseq, do_final)
    b.xB = [b.bufs(S // 512, "x") for _ in range(nseq)]
    b.oB = b.buf("out")
    nc = b.build()
    b.es.close()
    return nc


def rel_bucket_np(dist):
    n = np.maximum(dist, 0)
    nf = np.maximum(n, 1).astype(np.float32)
    large = 16 + (np.log(nf / np.float32(16)) / np.float32(math.log(128 / 16)) * np.float32(16)).astype(np.int32)
    large = np.minimum(large, 31)
    return np.where(n < 16, n, large)


def host_consts(rel_bias, layers):
    ss = np.arange(128)[:, None]
    v = np.arange(GW)[None, :]
    dist = v - ss
    idx = rel_bucket_np(dist)
    gt = np.transpose(rel_bias[idx], (2, 0, 1)).astype(np.float32)
    gt = np.where(dist[None] >= 0, gt, np.float32(NEG)).astype(np.float32)
    cb31 = np.broadcast_to(rel_bias[31][None, :], (128, 12)).astype(np.float32).copy()
    ident = np.eye(128, dtype=np.float32)
    tt = np.arange(128)[:, None]
    s2 = np.arange(128)[None, :]
    causneg = np.where(s2 <= tt, 0.0, NEG).astype(np.float32)
    lconst = np.zeros((len(layers), 128, 2), np.float32)
    for i, l in enumerate(layers):
        li_ = 0.8 - 0.6 * math.exp(-0.3 * l)
        lconst[i, :, 0] = 1.0 - li_
        lconst[i, :, 1] = li_
    return dict(gtab=np.ascontiguousarray(gt), cb31=cb31, ident=ident, causneg=causneg, lconst=lconst)


def make_in_maps(inp, layers, seq_groups, S):
    L = len(layers)
    ls = list(layers)
    c = host_consts(np.asarray(inp["rel_bias"], np.float32), ls)
    rep = lambda a: np.ascontiguousarray(np.broadcast_to(a[:, None, :], (a.shape[0], 128, a.shape[1])))
    common = dict(
        w_in=np.ascontiguousarray(inp["w_in"][ls]), w_out=np.ascontiguousarray(inp["w_out"][ls]),
        w_gate=np.ascontiguousarray(inp["w_gate"][ls]), w_up=np.ascontiguousarray(inp["w_up"][ls]),
        w_down=np.ascontiguousarray(inp["w_down"][ls]),
        gattn=np.ascontiguousarray(inp["attn_norm"][ls].reshape(L, 8, 128).transpose(0, 2, 1)),
        gffn=np.ascontiguousarray(inp["ffn_norm"][ls].reshape(L, 8, 128).transpose(0, 2, 1)),
        gfin=np.ascontiguousarray(inp["final_norm"].reshape(8, 128).T),
        lamrep=rep(inp["diff_lambda"][ls].reshape(L, 256)),
        sublnrep=rep(inp["diff_subln"][ls]),
        **c)
    maps = []
    for g in seq_groups:
        m = dict(common)
        m["xT"] = np.ascontiguousarray(np.transpose(inp["x"][g], (0, 2, 1)))
        maps.append(m)
    return maps


_PROG = {}


def kernel(**inputs):
    inp = {k: np.asarray(v, dtype=np.float32) for k, v in inputs.items()}
    B, S, _ = inp["x"].shape
    ncore = 8
    per = B // ncore
    layers = list(range(DEPTH))
    key = (S, tuple(layers), per)
    if key not in _PROG:
        _PROG[key] = make_program(S, layers, per, True)
    nc = _PROG[key]
    groups = [list(range(c * per, (c + 1) * per)) for c in range(ncore)]
    maps = make_in_maps(inp, layers, groups, S)
    res = run_bass_kernel_spmd(nc, maps, core_ids=list(range(ncore)))
    out = np.empty((B, S, D), np.float32)
    for c in range(ncore):
        o = res.results[c]["outT"]
        for i, b in enumerate(groups[c]):
            out[b] = o[i].T
    return out
```
